# Optimizing a Trainium2 kernel written in Bass

```python
import math
import jax
import jax.numpy as jnp
from jax import lax
import numpy as np

D_MODEL = 1024
BATCH = 8
SEQ = 4096
DEPTH = 2

GDN_HEADS = 4
GDN_HEAD_DIM = 128
GDN_WIDTH = GDN_HEADS * GDN_HEAD_DIM
GDN_CONV = 4
GDN_CHUNK = 64
S5_GROUP = 16
S5_GROUPS = 16
S5_WIDTH = S5_GROUPS * S5_GROUP
S5_STATE = 64
MOBA_HEADS = 4
MOBA_HEAD_DIM = 64
MOBA_WIDTH = MOBA_HEADS * MOBA_HEAD_DIM
MOBA_BLOCK = 256
MOBA_TOPK = 3
MOBA_Q_CHUNK = 64
N_BRANCH = 3
IN_SIZES = (3 * GDN_WIDTH, GDN_WIDTH, GDN_HEADS, GDN_HEADS, S5_WIDTH, 3 * MOBA_WIDTH, N_BRANCH * D_MODEL)
IN_COLS = 3 * GDN_WIDTH + GDN_WIDTH + 2 * GDN_HEADS + S5_WIDTH + 3 * MOBA_WIDTH + N_BRANCH * D_MODEL
PEER_HEADS = 8
PEER_NKEYS = 128
PEER_EXPERTS = PEER_NKEYS * PEER_NKEYS
PEER_QDIM = 256
PEER_TOPK = 16
PEER_TOKEN_CHUNK = 128
RMS_EPS = 1e-6
NEG_INF = -1e30

kernel_name = 'hybrid_gdn_s5_moba_peer_adaln'

F32 = jnp.float32


def _rmsnorm(x, w):
    xf = x.astype(F32)
    y = xf * lax.rsqrt(jnp.mean(xf * xf, axis=-1, keepdims=True) + RMS_EPS)
    return (y * w.astype(F32)).astype(x.dtype)


def _l2norm(t):
    return t * lax.rsqrt(jnp.sum(t * t, axis=-1, keepdims=True) + 1e-6)


def _causal_conv(x, w):
    k, ch = w.shape
    return lax.conv_general_dilated(
        x, w[:, None, :].astype(x.dtype), window_strides=(1,), padding=[(k - 1, 0)],
        dimension_numbers=('NWC', 'WIO', 'NWC'), feature_group_count=ch)


def _inv_unit_lower(a):
    n = -a
    eye = jnp.eye(a.shape[-1], dtype=a.dtype)
    t = eye + n
    p = n
    for _ in range(int(math.log2(a.shape[-1])) - 1):
        p = p @ p
        t = t + t @ p
    return t


def _gated_delta_rule(q, k, v, g, beta):
    bt, nh, L, dk = q.shape
    dv = v.shape[-1]
    C = GDN_CHUNK
    nc = L // C
    q, k, v = (t.reshape(bt, nh, nc, C, -1) for t in (q, k, v))
    g = jnp.cumsum(g.reshape(bt, nh, nc, C), axis=-1)
    beta = beta.reshape(bt, nh, nc, C)
    tril = jnp.tril(jnp.ones((C, C), bool))
    strict = jnp.tril(jnp.ones((C, C), bool), -1)
    diff = g[..., :, None] - g[..., None, :]
    decay = jnp.where(tril, jnp.exp(jnp.where(tril, diff, 0.0)), 0.0)
    k_beta = k * beta[..., None]
    a = jnp.where(strict, jnp.einsum('bhnid,bhnjd->bhnij', k_beta, k) * decay, 0.0)
    t_inv = _inv_unit_lower(a)
    u = t_inv @ (v * beta[..., None])
    w = t_inv @ (k_beta * jnp.exp(g)[..., None])
    qk = jnp.where(tril, jnp.einsum('bhnid,bhnjd->bhnij', q, k) * decay, 0.0)

    def step(state, xs):
        q_c, k_c, u_c, w_c, qk_c, g_c = xs
        v_new = u_c - jnp.einsum('bhck,bhkv->bhcv', w_c, state)
        o = (jnp.einsum('bhck,bhkv->bhcv', q_c * jnp.exp(g_c)[..., None], state)
             + jnp.einsum('bhcs,bhsv->bhcv', qk_c, v_new))
        g_last = g_c[..., -1:]
        state = (state * jnp.exp(g_last)[..., None]
                 + jnp.einsum('bhck,bhcv->bhkv', k_c * jnp.exp(g_last - g_c)[..., None], v_new))
        return state, o

    xs = tuple(jnp.moveaxis(t, 2, 0) for t in (q, k, u, w, qk, g))
    state0 = jnp.zeros((bt, nh, dk, dv), F32)
    _, o = lax.scan(step, state0, xs)
    return jnp.moveaxis(o, 0, 2).reshape(bt, nh, L, dv)


def _gdn_branch(qkv, z, b, a, conv_w, a_log, dt_bias, norm_w):
    bt, L, _ = qkv.shape
    qkv = jax.nn.silu(_causal_conv(qkv, conv_w))
    q, k, v = jnp.split(qkv, 3, axis=-1)
    heads = lambda t: t.reshape(bt, L, GDN_HEADS, GDN_HEAD_DIM).transpose(0, 2, 1, 3).astype(F32)
    q = _l2norm(heads(q)) * (GDN_HEAD_DIM ** -0.5)
    k = _l2norm(heads(k))
    v = heads(v)
    beta = jax.nn.sigmoid(b.astype(F32)).transpose(0, 2, 1)
    g = (-jnp.exp(a_log.astype(F32)) * jax.nn.softplus(a.astype(F32) + dt_bias.astype(F32))).transpose(0, 2, 1)
    o = _gated_delta_rule(q, k, v, g, beta).transpose(0, 2, 1, 3)
    zf = z.reshape(bt, L, GDN_HEADS, GDN_HEAD_DIM).astype(F32)
    o = o * lax.rsqrt(jnp.mean(o * o, axis=-1, keepdims=True) + RMS_EPS) * norm_w.astype(F32) * jax.nn.silu(zf)
    return o.reshape(bt, L, GDN_WIDTH).astype(qkv.dtype)


def _s5_branch(u, a_re, a_im, b_re, b_im, c_re, c_im, d, log_dt, glu_w, glu_b):
    bt, L, _ = u.shape
    uf = u.astype(F32)
    ug = uf.reshape(bt, L, S5_GROUPS, S5_GROUP)
    lam = lax.complex(a_re.astype(F32), a_im.astype(F32))
    dt = jnp.exp(log_dt.astype(F32))[:, None]
    a_bar = jnp.exp(lam * dt)
    b_bar = ((a_bar - 1.0) / lam)[..., None] * lax.complex(b_re.astype(F32), b_im.astype(F32))
    bu = lax.complex(jnp.einsum('gph,blgh->lbgp', jnp.real(b_bar), ug),
                     jnp.einsum('gph,blgh->lbgp', jnp.imag(b_bar), ug))
    a_seq = jnp.broadcast_to(a_bar, (L, 1) + a_bar.shape)

    def combine(e1, e2):
        a1, s1 = e1
        a2, s2 = e2
        return a1 * a2, a2 * s1 + s2

    _, states = lax.associative_scan(combine, (a_seq, bu), axis=0)
    y = (jnp.einsum('ghp,lbgp->blgh', c_re.astype(F32), jnp.real(states))
         - jnp.einsum('ghp,lbgp->blgh', c_im.astype(F32), jnp.imag(states)))
    y = y.reshape(bt, L, S5_WIDTH) + d.astype(F32) * uf
    zg = jax.nn.gelu(y, approximate=False)
    zz = zg @ glu_w.astype(F32) + glu_b.astype(F32)
    out = zz[..., :S5_WIDTH] * jax.nn.sigmoid(zz[..., S5_WIDTH:])
    return out.astype(u.dtype)


def _moba_attention(q, k, v):
    bt, nh, L, dh = q.shape
    n_blk = -(-L // MOBA_BLOCK)
    lp = n_blk * MOBA_BLOCK
    pad = [(0, 0), (0, 0), (0, lp - L), (0, 0)]
    q, k, v = (jnp.pad(t, pad).astype(F32) for t in (q, k, v))
    kb = k.reshape(bt, nh, n_blk, MOBA_BLOCK, dh)
    vb = v.reshape(bt, nh, n_blk, MOBA_BLOCK, dh)
    k_mean = jnp.mean(kb, axis=3)
    gate = jnp.einsum('bhld,bhnd->bhln', q, k_mean)
    q_blk = jnp.arange(lp) // MOBA_BLOCK
    past = jnp.arange(n_blk)[None, :] < q_blk[:, None]
    gate = jnp.where(past, gate, NEG_INF)
    n_sel = min(MOBA_TOPK, n_blk)
    _, sel = lax.top_k(gate, n_sel)
    scale = dh ** -0.5
    b_ix = jnp.arange(bt)[:, None, None, None]
    h_ix = jnp.arange(nh)[None, :, None, None]

    def one_chunk(ci):
        start = ci * MOBA_Q_CHUNK
        own = start // MOBA_BLOCK
        qc = lax.dynamic_slice_in_dim(q, start, MOBA_Q_CHUNK, axis=2)
        sc = lax.dynamic_slice_in_dim(sel, start, MOBA_Q_CHUNK, axis=2)
        k_sel = kb[b_ix, h_ix, sc]
        v_sel = vb[b_ix, h_ix, sc]
        s_sel = jnp.einsum('bhqd,bhqnsd->bhqns', qc, k_sel) * scale
        s_sel = jnp.where((sc < own)[..., None], s_sel, NEG_INF)
        k_own = lax.dynamic_index_in_dim(kb, own, axis=2, keepdims=False)
        v_own = lax.dynamic_index_in_dim(vb, own, axis=2, keepdims=False)
        s_own = jnp.einsum('bhqd,bhsd->bhqs', qc, k_own) * scale
        q_pos = start + jnp.arange(MOBA_Q_CHUNK)
        k_pos = own * MOBA_BLOCK + jnp.arange(MOBA_BLOCK)
        s_own = jnp.where(k_pos[None, :] <= q_pos[:, None], s_own, NEG_INF)
        s_all = jnp.concatenate([s_sel.reshape(bt, nh, MOBA_Q_CHUNK, n_sel * MOBA_BLOCK), s_own], axis=-1)
        p = jax.nn.softmax(s_all, axis=-1)
        p_sel = p[..., :n_sel * MOBA_BLOCK].reshape(bt, nh, MOBA_Q_CHUNK, n_sel, MOBA_BLOCK)
        p_own = p[..., n_sel * MOBA_BLOCK:]
        return (jnp.einsum('bhqns,bhqnsd->bhqd', p_sel, v_sel)
                + jnp.einsum('bhqs,bhsd->bhqd', p_own, v_own))

    out = lax.map(one_chunk, jnp.arange(lp // MOBA_Q_CHUNK))
    return jnp.moveaxis(out, 0, 2).reshape(bt, nh, lp, dh)[:, :, :L]


def _moba_branch(qkv):
    bt, L, _ = qkv.shape
    q, k, v = jnp.split(qkv, 3, axis=-1)
    heads = lambda t: t.reshape(bt, L, MOBA_HEADS, MOBA_HEAD_DIM).transpose(0, 2, 1, 3)
    o = _moba_attention(heads(q), heads(k), heads(v))
    return o.transpose(0, 2, 1, 3).reshape(bt, L, MOBA_WIDTH).astype(qkv.dtype)


def _hybrid_mixer(h, w_in, conv_w, a_log, dt_bias, gdn_norm_w,
                  s5_a_re, s5_a_im, s5_b_re, s5_b_im, s5_c_re, s5_c_im, s5_d, s5_log_dt,
                  s5_glu_w, s5_glu_b, w_branch_a, w_branch_b, w_branch_c, w_out):
    bt, L, _ = h.shape
    p = h @ w_in
    cuts = np.cumsum(IN_SIZES)[:-1].tolist()
    qkv_a, z_a, beta_a, alpha_a, u_b, qkv_c, gate_in = jnp.split(p, cuts, axis=-1)
    y_a = _gdn_branch(qkv_a, z_a, beta_a, alpha_a, conv_w, a_log, dt_bias, gdn_norm_w) @ w_branch_a
    y_b = _s5_branch(u_b, s5_a_re, s5_a_im, s5_b_re, s5_b_im, s5_c_re, s5_c_im, s5_d,
                     s5_log_dt, s5_glu_w, s5_glu_b) @ w_branch_b
    y_c = _moba_branch(qkv_c) @ w_branch_c
    gates = jax.nn.sigmoid(gate_in.astype(F32)).astype(h.dtype).reshape(bt, L, N_BRANCH, D_MODEL)
    merged = gates[:, :, 0] * y_a + gates[:, :, 1] * y_b + gates[:, :, 2] * y_c
    return merged @ w_out


def _peer(h, wq, k1, k2, u_tab, v_tab):
    bt, L, d = h.shape
    half = PEER_QDIM // 2
    q = (h @ wq).astype(F32).reshape(bt, L, PEER_HEADS, PEER_QDIM)
    s1 = jnp.einsum('blhd,hnd->blhn', q[..., :half], k1.astype(F32))
    s2 = jnp.einsum('blhd,hnd->blhn', q[..., half:], k2.astype(F32))
    v1, i1 = lax.top_k(s1, PEER_TOPK)
    v2, i2 = lax.top_k(s2, PEER_TOPK)
    n_cand = PEER_TOPK * PEER_TOPK
    cand_s = (v1[..., :, None] + v2[..., None, :]).reshape(bt, L, PEER_HEADS, n_cand)
    cand_i = (i1[..., :, None] * PEER_NKEYS + i2[..., None, :]).reshape(bt, L, PEER_HEADS, n_cand)
    top_s, pos = lax.top_k(cand_s, PEER_TOPK)
    expert = jnp.take_along_axis(cand_i, pos, axis=-1)
    gate = jax.nn.softmax(top_s, axis=-1)
    n_sel = PEER_HEADS * PEER_TOPK
    n_chunk = (bt * L) // PEER_TOKEN_CHUNK
    hs = h.reshape(n_chunk, PEER_TOKEN_CHUNK, d)
    es = expert.reshape(n_chunk, PEER_TOKEN_CHUNK, n_sel)
    gs = gate.reshape(n_chunk, PEER_TOKEN_CHUNK, n_sel)

    def chunk(args):
        hc, ec, gc = args
        act = jax.nn.gelu(jnp.einsum('td,tkd->tk', hc.astype(F32), u_tab[ec].astype(F32)), approximate=False)
        return jnp.einsum('tk,tkd->td', gc * act, v_tab[ec].astype(F32))

    out = lax.map(chunk, (hs, es, gs))
    return out.reshape(bt, L, d).astype(h.dtype)


def setup_inputs(seed: int = 0) -> dict:
    key = jax.random.key(seed)
    ks = iter(jax.random.split(key, 48))
    nrm = lambda shape, s: jax.random.normal(next(ks), shape, F32) * s
    D = D_MODEL
    L = DEPTH
    G, P = S5_GROUPS, S5_STATE
    x = nrm((BATCH, SEQ, D), 1.0)
    c = nrm((BATCH, D), 1.0)
    ada_w = nrm((L, D, 6 * D), 0.5 * D ** -0.5)
    ada_b = nrm((L, 6 * D), 0.02)
    norm1_w = 1.0 + nrm((L, D), 0.02)
    w_in = nrm((L, D, IN_COLS), D ** -0.5)
    gdn_conv_w = nrm((L, GDN_CONV, 3 * GDN_WIDTH), GDN_CONV ** -0.5)
    gdn_a_log = jnp.log(jax.random.uniform(next(ks), (L, GDN_HEADS), F32, 1.0, 16.0))
    dt0 = jnp.exp(jax.random.uniform(next(ks), (L, GDN_HEADS), F32, math.log(1e-3), math.log(1e-1)))
    gdn_dt_bias = dt0 + jnp.log(-jnp.expm1(-dt0))
    gdn_norm_w = 1.0 + nrm((L, GDN_HEAD_DIM), 0.02)
    s5_a_re = -0.5 * jnp.exp(nrm((L, G, P), 0.02))
    s5_a_im = jnp.pi * jnp.arange(P, dtype=F32) + nrm((L, G, P), 0.02)
    s5_b_re = nrm((L, G, P, S5_GROUP), (2 * S5_GROUP) ** -0.5)
    s5_b_im = nrm((L, G, P, S5_GROUP), (2 * S5_GROUP) ** -0.5)
    s5_c_re = nrm((L, G, S5_GROUP, P), (2 * P) ** -0.5)
    s5_c_im = nrm((L, G, S5_GROUP, P), (2 * P) ** -0.5)
    s5_d = nrm((L, S5_WIDTH), 1.0)
    s5_log_dt = jax.random.uniform(next(ks), (L, G), F32, math.log(1e-3), math.log(1e-1))
    s5_glu_w = nrm((L, S5_WIDTH, 2 * S5_WIDTH), S5_WIDTH ** -0.5)
    s5_glu_b = nrm((L, 2 * S5_WIDTH), 0.02)
    w_branch_a = nrm((L, GDN_WIDTH, D), GDN_WIDTH ** -0.5)
    w_branch_b = nrm((L, S5_WIDTH, D), S5_WIDTH ** -0.5)
    w_branch_c = nrm((L, MOBA_WIDTH, D), MOBA_WIDTH ** -0.5)
    w_out = nrm((L, D, D), D ** -0.5)
    norm2_w = 1.0 + nrm((L, D), 0.02)
    peer_wq = nrm((L, D, PEER_HEADS * PEER_QDIM), D ** -0.5)
    peer_k1 = nrm((L, PEER_HEADS, PEER_NKEYS, PEER_QDIM // 2), (PEER_QDIM // 2) ** -0.5)
    peer_k2 = nrm((L, PEER_HEADS, PEER_NKEYS, PEER_QDIM // 2), (PEER_QDIM // 2) ** -0.5)
    peer_u = nrm((L, PEER_EXPERTS, D), D ** -0.5)
    peer_v = nrm((L, PEER_EXPERTS, D), 0.5)
    final_norm_w = 1.0 + nrm((D,), 0.02)
    return {'x': x, 'c': c, 'ada_w': ada_w, 'ada_b': ada_b, 'norm1_w': norm1_w, 'w_in': w_in,
            'gdn_conv_w': gdn_conv_w, 'gdn_a_log': gdn_a_log, 'gdn_dt_bias': gdn_dt_bias,
            'gdn_norm_w': gdn_norm_w, 's5_a_re': s5_a_re, 's5_a_im': s5_a_im, 's5_b_re': s5_b_re,
            's5_b_im': s5_b_im, 's5_c_re': s5_c_re, 's5_c_im': s5_c_im, 's5_d': s5_d,
            's5_log_dt': s5_log_dt, 's5_glu_w': s5_glu_w, 's5_glu_b': s5_glu_b,
            'w_branch_a': w_branch_a, 'w_branch_b': w_branch_b, 'w_branch_c': w_branch_c,
            'w_out': w_out, 'norm2_w': norm2_w, 'peer_wq': peer_wq, 'peer_k1': peer_k1,
            'peer_k2': peer_k2, 'peer_u': peer_u, 'peer_v': peer_v, 'final_norm_w': final_norm_w}


def reference(x, c, ada_w, ada_b, norm1_w, w_in, gdn_conv_w, gdn_a_log, gdn_dt_bias, gdn_norm_w,
              s5_a_re, s5_a_im, s5_b_re, s5_b_im, s5_c_re, s5_c_im, s5_d, s5_log_dt, s5_glu_w,
              s5_glu_b, w_branch_a, w_branch_b, w_branch_c, w_out, norm2_w, peer_wq, peer_k1,
              peer_k2, peer_u, peer_v, final_norm_w):
    for l in range(DEPTH):
        mod = jax.nn.silu(c) @ ada_w[l] + ada_b[l]
        sh1, sc1, g1, sh2, sc2, g2 = jnp.split(mod[:, None, :], 6, axis=-1)
        h = _rmsnorm(x, norm1_w[l]) * (1.0 + sc1) + sh1
        x = x + g1 * _hybrid_mixer(h, w_in[l], gdn_conv_w[l], gdn_a_log[l], gdn_dt_bias[l], gdn_norm_w[l],
                                   s5_a_re[l], s5_a_im[l], s5_b_re[l], s5_b_im[l], s5_c_re[l], s5_c_im[l],
                                   s5_d[l], s5_log_dt[l], s5_glu_w[l], s5_glu_b[l],
                                   w_branch_a[l], w_branch_b[l], w_branch_c[l], w_out[l])
        h = _rmsnorm(x, norm2_w[l]) * (1.0 + sc2) + sh2
        x = x + g2 * _peer(h, peer_wq[l], peer_k1[l], peer_k2[l], peer_u[l], peer_v[l])
    return _rmsnorm(x, final_norm_w)
```

```python
import contextlib
import numpy as np
import concourse.bass as bass
import concourse.mybir as mybir
from concourse.bass_utils import run_bass_kernel_spmd

F32 = mybir.dt.float32
BF16 = mybir.dt.bfloat16
AF = mybir.ActivationFunctionType
ALU = mybir.AluOpType
AX = mybir.AxisListType

ENGS = ['pe', 'dve', 'act', 'pool', 'sp']
L = 4096
D = 1024
NTILE = 32
BIGRAW = 240000.0


class View:
    __slots__ = ('b', 'ap')

    def __init__(self, b, ap):
        self.b = b
        self.ap = ap

    def bc(self, shape):
        return View(self.b, self.ap.broadcast_to(list(shape)))

    def re(self, pat, **kw):
        return View(self.b, self.ap.rearrange(pat, **kw))

    def __getitem__(self, idx):
        return View(self.b, self.ap[idx])


class Buf:
    __slots__ = ('t', 'w', 'r', 'name', 'ps')

    def __init__(self, t=None, name=''):
        self.t = t
        self.ps = False
        self.w = {}
        self.r = {}
        self.name = name

    def __getitem__(self, idx):
        return View(self, self.t[idx])

    def re(self, pat, **kw):
        return View(self, self.t.rearrange(pat, **kw))

    def alias(self):
        b = Buf(self.t, self.name + '_a')
        b.ps = self.ps
        return b


class Prog:
    def __init__(self, nc, n_dma_sems=24):
        self.nc = nc
        self.es = contextlib.ExitStack()
        self.ses = None
        self.ops = {e: [] for e in ENGS}
        self.cnt = {e: 0 for e in ENGS}
        self.sem = {}
        for e in ['pe', 'dve', 'act', 'pool']:
            self.sem[e] = self.es.enter_context(nc.semaphore('s_' + e))
        self.dsem = [self.es.enter_context(nc.semaphore('d%d' % i)) for i in range(n_dma_sems)]
        self.dcum = [0] * n_dma_sems
        self.dnext = 0
        self.known = {e: {} for e in ENGS}
        self._uid = 0
        self.ninstr = 0

    def sbuf(self, shape, dt, name=None, persist=False):
        self._uid += 1
        name = name or ('sb%d' % self._uid)
        es = self.es if (persist or self.ses is None) else self.ses
        t = es.enter_context(self.nc.sbuf_tensor(name, list(shape), dt))
        return Buf(t, name)

    def psum(self, shape, dt=F32, name=None):
        self._uid += 1
        name = name or ('ps%d' % self._uid)
        es = self.es if self.ses is None else self.ses
        t = es.enter_context(self.nc.psum_tensor(name, list(shape), dt))
        b = Buf(t, name)
        b.ps = True
        return b

    def dram(self, name, shape, dt, kind='Internal'):
        t = self.nc.dram_tensor(name, list(shape), dt, kind=kind)
        return Buf(t.ap(), name)

    def _semobj(self, key):
        return self.sem[key] if isinstance(key, str) else self.dsem[key]

    def _collect(self, eng, reads, writes):
        need = {}
        for b in reads:
            for k, v in b.w.items():
                if need.get(k, 0) < v:
                    need[k] = v
            if b.ps:
                for k, v in b.r.items():
                    if k != eng and need.get(k, 0) < v:
                        need[k] = v
        for b in writes:
            for k, v in b.w.items():
                if need.get(k, 0) < v:
                    need[k] = v
            for k, v in b.r.items():
                if need.get(k, 0) < v:
                    need[k] = v
        waits = []
        kn = self.known[eng]
        for k, v in need.items():
            if k == 'pe' and eng == 'pe':
                continue
            if kn.get(k, 0) >= v:
                continue
            kn[k] = v
            waits.append((k, v))
        return waits

    def op(self, eng, fn, reads=(), writes=()):
        waits = self._collect(eng, reads, writes)
        self.cnt[eng] += 1
        idx = self.cnt[eng]
        self.ops[eng].append((waits, fn, (eng, 1)))
        for b in writes:
            b.w = {eng: idx}
            b.r = {}
        for b in reads:
            if b.r.get(eng, 0) < idx:
                b.r[eng] = idx
        self.ninstr += 1

    def dma(self, eng, out, in_, **kw):
        reads, writes = [in_.b], [out.b]
        waits = self._collect(eng, reads, writes)
        k = self.dnext
        self.dnext = (self.dnext + 1) % len(self.dsem)
        if self.dcum[k] > 0 and self.known[eng].get(k, 0) < self.dcum[k]:
            self.known[eng][k] = self.dcum[k]
            waits.append((k, self.dcum[k]))
        self.dcum[k] += 16
        val = self.dcum[k]
        oa, ia = out.ap, in_.ap

        def fn(e):
            return e.dma_start(out=oa, in_=ia, **kw)
        self.ops[eng].append((waits, fn, (k, 16)))
        for b in writes:
            b.w = {k: val}
            b.r = {}
        for b in reads:
            if b.r.get(k, 0) < val:
                b.r[k] = val
        self.ninstr += 1

    @contextlib.contextmanager
    def stage(self):
        self.ses = contextlib.ExitStack()
        try:
            yield
            self.flush()
        finally:
            self.ses.close()
            self.ses = None

    def flush(self):
        waits = []
        for k in range(len(self.dsem)):
            if self.dcum[k] > 0 and self.known['sp'].get(k, 0) < self.dcum[k]:
                self.known['sp'][k] = self.dcum[k]
                waits.append((k, self.dcum[k]))
        self.ops['sp'].append((waits, None, None))
        nc = self.nc
        with nc.Block() as block:
            def mk(ename):
                def body(e):
                    for waits, fn, inc in self.ops[ename]:
                        for k, v in waits:
                            e.wait_ge(self._semobj(k), v)
                        if fn is None:
                            continue
                        ins = fn(e)
                        ins.then_inc(self._semobj(inc[0]), inc[1])
                return body
            block.tensor(mk('pe'))
            block.vector(mk('dve'))
            block.scalar(mk('act'))
            block.gpsimd(mk('pool'))
            block.sync(mk('sp'))
        self.ops = {e: [] for e in ENGS}
        for e in ENGS:
            for e2 in ['pe', 'dve', 'act', 'pool']:
                self.known[e][e2] = self.cnt[e2]
            for k in range(len(self.dsem)):
                self.known[e][k] = self.dcum[k]

    def close(self):
        self.es.close()

    def mm(self, out, lhsT, rhs, start=True, stop=True):
        self.op('pe', lambda e: e.matmul(out.ap, lhsT=lhsT.ap, rhs=rhs.ap, start=start, stop=stop),
                reads=[lhsT.b, rhs.b], writes=[out.b])

    def tr(self, out, in_, ident):
        self.op('pe', lambda e: e.transpose(out.ap, in_.ap, ident.ap), reads=[in_.b, ident.b], writes=[out.b])

    def act(self, out, in_, func, bias=None, scale=None, accum=None):
        reads = [in_.b]
        kw = {}
        if bias is not None:
            if isinstance(bias, View):
                reads.append(bias.b)
                kw['bias'] = bias.ap
            else:
                kw['bias'] = float(bias)
        if scale is not None:
            if isinstance(scale, View):
                reads.append(scale.b)
                kw['scale'] = scale.ap
            else:
                kw['scale'] = float(scale)
        if func == AF.Copy and isinstance(scale, View):
            func = AF.Identity
        writes = [out.b]
        if accum is not None:
            kw['accum_out'] = accum.ap
            writes.append(accum.b)
        self.op('act', lambda e: e.activation(out=out.ap, in_=in_.ap, func=func, **kw), reads=reads, writes=writes)

    def tt(self, eng, out, a, b, op):
        self.op(eng, lambda e: e.tensor_tensor(out=out.ap, in0=a.ap, in1=b.ap, op=op), reads=[a.b, b.b], writes=[out.b])

    def ts(self, eng, out, a, s1, op0, s2=None, op1=None):
        reads = [a.b]
        s1v = s1.ap if isinstance(s1, View) else float(s1)
        if isinstance(s1, View):
            reads.append(s1.b)
        s2v = None
        if s2 is not None:
            s2v = s2.ap if isinstance(s2, View) else float(s2)
            if isinstance(s2, View):
                reads.append(s2.b)
        if op1 is None:
            self.op(eng, lambda e: e.tensor_scalar(out=out.ap, in0=a.ap, scalar1=s1v, scalar2=None, op0=op0),
                    reads=reads, writes=[out.b])
        else:
            self.op(eng, lambda e: e.tensor_scalar(out=out.ap, in0=a.ap, scalar1=s1v, scalar2=s2v, op0=op0, op1=op1),
                    reads=reads, writes=[out.b])

    def stt(self, out, in0, scalar, in1, op0, op1):
        reads = [in0.b, in1.b]
        sv = scalar.ap if isinstance(scalar, View) else float(scalar)
        if isinstance(scalar, View):
            reads.append(scalar.b)
        self.op('dve', lambda e: e.scalar_tensor_tensor(out=out.ap, in0=in0.ap, scalar=sv, in1=in1.ap, op0=op0, op1=op1),
                reads=reads, writes=[out.b])

    def cp(self, eng, out, in_):
        if eng == 'act':
            self.op('act', lambda e: e.activation(out=out.ap, in_=in_.ap, func=AF.Copy), reads=[in_.b], writes=[out.b])
        else:
            self.op(eng, lambda e: e.tensor_copy(out=out.ap, in_=in_.ap), reads=[in_.b], writes=[out.b])

    def memset(self, eng, out, val):
        self.op(eng, lambda e: e.memset(out.ap, val), writes=[out.b])

    def recip(self, out, in_):
        self.op('dve', lambda e: e.reciprocal(out=out.ap, in_=in_.ap), reads=[in_.b], writes=[out.b])


def _const_mats():
    idx = np.arange(128)
    same = (idx[:, None] // 64) == (idx[None, :] // 64)
    ident = np.eye(128, dtype=np.float32)
    L2 = (same & (idx[:, None] >= idx[None, :])).astype(np.float32)
    SL2 = (same & (idx[:, None] > idx[None, :])).astype(np.float32)
    U2 = L2.T.copy()
    B2 = same.astype(np.float32)
    ones = np.ones((128, 128), np.float32)
    cm = np.stack([ident, L2, SL2, U2, B2, ones], axis=1)
    return np.ascontiguousarray(cm)


C_ID, C_L2, C_SL2, C_U2, C_B2, C_ONES = range(6)


def _os_env(k):
    import os
    return os.environ.get(k)


def build(upto=99, dbg=(), branches=(0, 1, 2, 3), fake_pt=False, fake_xm=False, peer_ntb=16, peer_only=False):
    nc = bass.Bass('TRN2', target_bir_lowering=False)
    P = Prog(nc)
    ein = lambda n, s, dt=F32: P.dram(n, s, dt, kind='ExternalInput')
    xT = ein('xT', [D, L])
    cT = ein('cT', [128, 8])
    cmats = ein('cmats', [128, 6, 128])
    cvec = ein('cvec', [128, 4])
    fnwT = ein('fnwT', [128, 8])
    cmaskD = ein('cmaskD', [128, 2, 256])
    selTD = ein('selTD', [128, 16, 128])
    W = []
    for l in range(2):
        w = {}
        s = '_%d' % l
        w['ada_w'] = ein('ada_w' + s, [D, 6 * D])
        w['ada_bT'] = ein('ada_bT' + s, [128, 48])
        w['n1T'] = ein('n1T' + s, [128, 8])
        w['n2T'] = ein('n2T' + s, [128, 8])
        w['w_main'] = ein('w_main' + s, [D, 6144])
        w['w_ba'] = ein('w_ba' + s, [D, 8])
        for nm in ['s5_are', 's5_aim', 's5_ldt']:
            w[nm] = ein(nm + s, [128, 8])
        w['s5_bre'] = ein('s5_bre' + s, [128, 8, 16])
        w['s5_bim'] = ein('s5_bim' + s, [128, 8, 16])
        w['s5_ctre'] = ein('s5_ctre' + s, [16, 64, 16])
        w['s5_ctim'] = ein('s5_ctim' + s, [16, 64, 16])
        w['s5_dT'] = ein('s5_dT' + s, [128, 2])
        w['s5_gluw'] = ein('s5_gluw' + s, [256, 512])
        w['s5_glubT'] = ein('s5_glubT' + s, [128, 4])
        w['wq'] = ein('wq' + s, [D, 2048])
        w['k1T'] = ein('k1T' + s, [8, 128, 128])
        w['k2T'] = ein('k2T' + s, [8, 128, 128])
        w['uT'] = ein('uT' + s, [D, 16384])
        w['vtab'] = ein('vtab' + s, [16384, D])
        w['convT'] = ein('convT' + s, [128, 12, 4])
        w['gdn_ab'] = ein('gdn_ab' + s, [128, 8])
        w['gnw'] = ein('gnw' + s, [128, 1])
        w['wa'] = ein('wa' + s, [512, D])
        w['wb'] = ein('wb' + s, [256, D])
        w['wc'] = ein('wc' + s, [256, D])
        w['wout'] = ein('wout' + s, [D, D])
        W.append(w)
    outT = P.dram('outT', [D, L], F32, kind='ExternalOutput')
    dbg_out = {}

    def dbg_dram(name, shape, dt):
        if name in dbg:
            b = P.dram('dbg_' + name, shape, dt, kind='ExternalOutput')
            dbg_out[name] = b
            return b
        return P.dram('scr_' + name, shape, dt)

    mod = [P.sbuf([128, 48], F32, persist=True) for _ in range(2)]
    A1 = [P.sbuf([128, 8], F32, persist=True) for _ in range(2)]
    A2 = [P.sbuf([128, 8], F32, persist=True) for _ in range(2)]
    cm = P.sbuf([128, 6, 128], F32, persist=True)
    cv = P.sbuf([128, 4], F32, persist=True)
    identb = P.sbuf([128, 128], BF16, persist=True)
    onesb = P.sbuf([128, 128], BF16, persist=True)

    with P.stage():
        P.dma('sp', cm[:, :, :], cmats[:, :, :])
        P.dma('sp', cv[:, :], cvec[:, :])
        P.cp('dve', identb[:, :], cm[:, C_ID, :])
        P.cp('dve', onesb[:, :], cm[:, C_ONES, :])
        ct = P.sbuf([128, 8], F32)
        sc_ = P.sbuf([128, 8], F32)
        P.dma('sp', ct[:, :], cT[:, :])
        P.act(sc_[:, :], ct[:, :], AF.Silu)
        wbuf = [P.sbuf([128, 8, 1024], F32) for _ in range(2)]
        mps = P.psum([128, 512])
        for l in range(2):
            bT = P.sbuf([128, 48], F32)
            n1 = P.sbuf([128, 8], F32)
            n2 = P.sbuf([128, 8], F32)
            P.dma('sp', bT[:, :], W[l]['ada_bT'][:, :])
            P.dma('sp', n1[:, :], W[l]['n1T'][:, :])
            P.dma('sp', n2[:, :], W[l]['n2T'][:, :])
            awv = W[l]['ada_w'].re('(kc p) j -> p kc j', p=128)
            for jg in range(6):
                wb = wbuf[jg % 2]
                P.dma('sp', wb[:, :, :], awv[:, :, jg * 1024:(jg + 1) * 1024])
                for jc in range(8):
                    col = jg * 8 + jc
                    for kc in range(8):
                        P.mm(mps[:, col:col + 1], wb[:, kc, jc * 128:(jc + 1) * 128], sc_[:, kc:kc + 1],
                             start=(kc == 0), stop=(kc == 7))
            P.tt('dve', mod[l][:, :], mps[:, 0:48], bT[:, :], ALU.add)
            P.stt(A1[l][:, :], mod[l][:, 8:16], 1.0, n1[:, :], ALU.add, ALU.mult)
            P.stt(A2[l][:, :], mod[l][:, 32:40], 1.0, n2[:, :], ALU.add, ALU.mult)
        if 'mod' in dbg:
            dm = dbg_dram('mod', [128, 96], F32)
            P.dma('sp', dm[:, 0:48], mod[0][:, :])
            P.dma('sp', dm[:, 48:96], mod[1][:, :])

    def norm_mod(Xsrc, A, B, hT_blks, hT):
        xv = Xsrc.re('(kc p) t -> p kc t', p=128)
        xts = [P.sbuf([128, 8, 512], F32) for _ in range(2)]
        sq = P.sbuf([128, 8, 512], F32)
        rs = P.sbuf([128, 512], F32)
        tmp = [P.sbuf([128, 512], F32) for _ in range(2)]
        ssp = P.psum([128, 512])
        for tb in range(8):
            xt = xts[tb % 2]
            P.dma('sp', xt[:, :, :], xv[:, :, tb * 512:(tb + 1) * 512])
            P.act(sq[:, :, :], xt[:, :, :], AF.Square)
            for kc in range(8):
                P.mm(ssp[:, :], cm[:, C_ONES, :], sq[:, kc, :], start=(kc == 0), stop=(kc == 7))
            P.act(rs[:, :], ssp[:, :], AF.Sqrt, bias=1e-6, scale=1.0 / D)
            P.recip(rs[:, :], rs[:, :])
            hb = hT_blks[tb]
            for kc in range(8):
                t_ = tmp[kc % 2]
                P.stt(t_[:, :], xt[:, kc, :], A[:, kc:kc + 1], rs[:, :], ALU.mult, ALU.mult)
                P.act(View(hb, hT.t[:, kc, tb * 512:(tb + 1) * 512]), t_[:, :], AF.Identity, bias=B[:, kc:kc + 1])

    TWO_PI = 2.0 * np.pi

    def s5_stage(l, PT, S5O):
        w = W[l]
        with P.stage():
            sm = lambda: P.sbuf([128, 8], F32)
            are, aim, ldt = sm(), sm(), sm()
            P.dma('sp', are[:, :], w['s5_are'][:, :])
            P.dma('sp', aim[:, :], w['s5_aim'][:, :])
            P.dma('sp', ldt[:, :], w['s5_ldt'][:, :])
            dt, lr, th, r = sm(), sm(), sm(), sm()
            P.act(dt[:, :], ldt[:, :], AF.Exp)
            P.tt('dve', lr[:, :], are[:, :], dt[:, :], ALU.mult)
            P.tt('dve', th[:, :], aim[:, :], dt[:, :], ALU.mult)
            P.act(r[:, :], lr[:, :], AF.Exp)
            ki = P.sbuf([128, 8], mybir.dt.int32)

            def sin_of(src, dst):
                t0, t1, t2 = sm(), sm(), sm()
                P.ts('dve', t0[:, :], src[:, :], 1.0 / TWO_PI, ALU.mult)
                P.cp('dve', ki[:, :], t0[:, :])
                P.cp('dve', t1[:, :], ki[:, :])
                P.stt(t2[:, :], t1[:, :], -TWO_PI, src[:, :], ALU.mult, ALU.add)
                P.ts('dve', t0[:, :], t2[:, :], float(np.pi), ALU.is_gt)
                P.stt(t1[:, :], t0[:, :], -TWO_PI, t2[:, :], ALU.mult, ALU.add)
                P.ts('dve', t0[:, :], t1[:, :], float(-np.pi), ALU.is_lt)
                P.stt(t2[:, :], t0[:, :], TWO_PI, t1[:, :], ALU.mult, ALU.add)
                P.act(dst[:, :], t2[:, :], AF.Sin)
            sn, cs, thc = sm(), sm(), sm()
            sin_of(th, sn)
            P.ts('dve', thc[:, :], th[:, :], float(np.pi / 2), ALU.add)
            sin_of(thc, cs)
            ar, ai, arm1, den, cre, cim, ncim, t0, t1 = sm(), sm(), sm(), sm(), sm(), sm(), sm(), sm(), sm()
            P.tt('dve', ar[:, :], r[:, :], cs[:, :], ALU.mult)
            P.tt('dve', ai[:, :], r[:, :], sn[:, :], ALU.mult)
            P.ts('dve', arm1[:, :], ar[:, :], -1.0, ALU.add)
            P.tt('dve', t0[:, :], are[:, :], are[:, :], ALU.mult)
            P.tt('dve', t1[:, :], aim[:, :], aim[:, :], ALU.mult)
            P.tt('dve', den[:, :], t0[:, :], t1[:, :], ALU.add)
            P.recip(den[:, :], den[:, :])
            P.tt('dve', t0[:, :], arm1[:, :], are[:, :], ALU.mult)
            P.tt('dve', t1[:, :], ai[:, :], aim[:, :], ALU.mult)
            P.tt('dve', t0[:, :], t0[:, :], t1[:, :], ALU.add)
            P.tt('dve', cre[:, :], t0[:, :], den[:, :], ALU.mult)
            P.tt('dve', t0[:, :], ai[:, :], are[:, :], ALU.mult)
            P.tt('dve', t1[:, :], arm1[:, :], aim[:, :], ALU.mult)
            P.tt('dve', t0[:, :], t0[:, :], t1[:, :], ALU.subtract)
            P.tt('dve', cim[:, :], t0[:, :], den[:, :], ALU.mult)
            P.ts('dve', ncim[:, :], cim[:, :], -1.0, ALU.mult)
            Bre = P.sbuf([128, 8, 16], F32)
            Bim = P.sbuf([128, 8, 16], F32)
            P.dma('sp', Bre[:, :, :], w['s5_bre'][:, :, :])
            P.dma('sp', Bim[:, :, :], w['s5_bim'][:, :, :])
            bbre = P.sbuf([128, 8, 16], F32)
            bbim = P.sbuf([128, 8, 16], F32)
            tb16 = P.sbuf([128, 16], F32)
            for s_ in range(8):
                P.ts('dve', tb16[:, :], Bre[:, s_, :], cre[:, s_:s_ + 1], ALU.mult)
                P.stt(bbre[:, s_, :], Bim[:, s_, :], ncim[:, s_:s_ + 1], tb16[:, :], ALU.mult, ALU.add)
                P.ts('dve', tb16[:, :], Bim[:, s_, :], cre[:, s_:s_ + 1], ALU.mult)
                P.stt(bbim[:, s_, :], Bre[:, s_, :], cim[:, s_:s_ + 1], tb16[:, :], ALU.mult, ALU.add)
            BbT = [[P.sbuf([128, 128], F32) for _ in range(8)] for _ in range(2)]
            CT = [[P.sbuf([128, 128], F32) for _ in range(8)] for _ in range(2)]
            stg = [P.sbuf([16, 128], F32) for _ in range(2)]
            psA = P.psum([128, 512])
            psB = P.psum([128, 512])
            for ri, bb in enumerate([bbre, bbim]):
                for s_ in range(8):
                    P.memset('pool', BbT[ri][s_][:, :], 0.0)
                    P.memset('pool', CT[ri][s_][:, :], 0.0)
                    st = stg[s_ % 2]
                    P.tr(psA[0:16, 0:128], bb[:, s_, :], cm[:, C_ID, :])
                    P.cp('act', st[:, :], psA[0:16, 0:128])
                    for g2 in range(2):
                        g = 2 * s_ + g2
                        r0 = (g % 8) * 16
                        P.dma('sp', BbT[ri][s_][r0:r0 + 16, g2 * 64:(g2 + 1) * 64], st[0:16, g2 * 64:(g2 + 1) * 64])
                        src = w['s5_ctre' if ri == 0 else 's5_ctim']
                        P.dma('sp', CT[ri][s_][g2 * 64:(g2 + 1) * 64, r0:r0 + 16], src[g, :, :])
            for s_ in range(8):
                P.ts('dve', CT[1][s_][:, :], CT[1][s_][:, :], -1.0, ALU.mult)
            Cj = [P.sbuf([128, 512], F32) for _ in range(8)]
            Sj = [P.sbuf([128, 512], F32) for _ in range(8)]
            rfull = [P.sbuf([128, 512], F32) for _ in range(8)]
            nsn = sm()
            tmpA = P.sbuf([128, 256], F32)
            tmpB = P.sbuf([128, 256], F32)
            for s_ in range(8):
                P.cp('dve', Cj[s_][:, 0:1], cs[:, s_:s_ + 1])
                P.cp('dve', Sj[s_][:, 0:1], sn[:, s_:s_ + 1])
                P.memset('pool', rfull[s_][:, :], 0.0)
                P.ts('pool', rfull[s_][:, :], rfull[s_][:, :], r[:, s_:s_ + 1], ALU.add)
                n = 1
                while n < 512:
                    c_n = Cj[s_][:, n - 1:n]
                    s_n = Sj[s_][:, n - 1:n]
                    P.ts('dve', nsn[:, 0:1], s_n, -1.0, ALU.mult)
                    P.ts('dve', tmpA[:, 0:n], Cj[s_][:, 0:n], c_n, ALU.mult)
                    P.ts('dve', tmpB[:, 0:n], Cj[s_][:, 0:n], s_n, ALU.mult)
                    P.stt(Cj[s_][:, n:2 * n], Sj[s_][:, 0:n], nsn[:, 0:1], tmpA[:, 0:n], ALU.mult, ALU.add)
                    P.stt(Sj[s_][:, n:2 * n], Sj[s_][:, 0:n], c_n, tmpB[:, 0:n], ALU.mult, ALU.add)
                    n *= 2
            ub = P.sbuf([128, 2, L], BF16)
            uF = P.sbuf([128, 2, L], F32)
            P.dma('sp', ub[:, :, :], PT.re('(c p) t -> p c t', p=128)[:, 16:18, :])
            P.cp('pool', uF[:, 0, :], ub[:, 0, :])
            P.cp('act', uF[:, 1, :], ub[:, 1, :])
            dcol = P.sbuf([128, 2], F32)
            P.dma('sp', dcol[:, :], w['s5_dT'][:, :])
            gwf = P.sbuf([128, 2, 512], F32)
            gw = P.sbuf([128, 2, 512], BF16)
            P.dma('sp', gwf[:, :, :], w['s5_gluw'].re('(kc p) j -> p kc j', p=128))
            P.cp('dve', gw[:, :, :], gwf[:, :, :])
            gb = P.sbuf([128, 4], F32)
            P.dma('sp', gb[:, :], w['s5_glubT'][:, :])
            Sre = [P.sbuf([128, 512], F32) for _ in range(8)]
            Sim = [P.sbuf([128, 512], F32) for _ in range(8)]
            wk = [P.sbuf([128, 512], F32) for _ in range(8)]
            psC = P.psum([128, 512])
            psD = P.psum([128, 512])
            psY = P.psum([128, 512])
            psG = [P.psum([128, 512]) for _ in range(2)]
            yv = P.sbuf([128, 512], F32)
            zg = P.sbuf([128, 2, 512], BF16)
            sig = P.sbuf([128, 512], F32)
            so = [P.sbuf([128, 2, 512], BF16) for _ in range(2)]
            s5v = S5O.re('(c p) t -> p c t', p=128)
            for tb in range(8):
                tsl = slice(tb * 512, (tb + 1) * 512)
                for s_ in range(8):
                    ch = s_ // 4
                    p1, p2 = (psA, psB) if s_ % 2 == 0 else (psC, psD)
                    P.mm(p1[:, :], BbT[0][s_][:, :], uF[:, ch, tsl])
                    P.mm(p2[:, :], BbT[1][s_][:, :], uF[:, ch, tsl])
                    t1, t2, t3, t4, zre, zim, hre, him = wk
                    P.tt('dve', t1[:, :], p1[:, :], Cj[s_][:, :], ALU.mult)
                    P.tt('dve', t2[:, :], p2[:, :], Sj[s_][:, :], ALU.mult)
                    P.tt('pool', zre[:, :], t1[:, :], t2[:, :], ALU.add)
                    P.tt('dve', t3[:, :], p2[:, :], Cj[s_][:, :], ALU.mult)
                    P.tt('dve', t4[:, :], p1[:, :], Sj[s_][:, :], ALU.mult)
                    P.tt('pool', zim[:, :], t3[:, :], t4[:, :], ALU.subtract)
                    for (z, h, Sx) in ((zre, hre, Sre[s_]), (zim, him, Sim[s_])):
                        if tb == 0:
                            P.op('dve', lambda e, h=h, z=z, rf=rfull[s_]: e.tensor_tensor_scan(
                                out=h.t[:, :], data0=rf.t[:, :], data1=z.t[:, :], initial=0.0, op0=ALU.mult, op1=ALU.add),
                                reads=[rfull[s_], z], writes=[h])
                        else:
                            P.op('dve', lambda e, h=h, z=z, rf=rfull[s_], Sx=Sx: e.tensor_tensor_scan(
                                out=h.t[:, :], data0=rf.t[:, :], data1=z.t[:, :], initial=Sx.t[:, 511:512],
                                op0=ALU.mult, op1=ALU.add), reads=[rfull[s_], z, Sx], writes=[h])
                    P.tt('dve', t1[:, :], hre[:, :], Cj[s_][:, :], ALU.mult)
                    P.tt('dve', t2[:, :], him[:, :], Sj[s_][:, :], ALU.mult)
                    P.tt('pool', Sre[s_][:, :], t1[:, :], t2[:, :], ALU.subtract)
                    P.tt('dve', t3[:, :], hre[:, :], Sj[s_][:, :], ALU.mult)
                    P.tt('dve', t4[:, :], him[:, :], Cj[s_][:, :], ALU.mult)
                    P.tt('pool', Sim[s_][:, :], t3[:, :], t4[:, :], ALU.add)
                for ch in range(2):
                    k = 0
                    for s_ in range(ch * 4, ch * 4 + 4):
                        P.mm(psY[:, :], CT[0][s_][:, :], Sre[s_][:, :], start=(k == 0), stop=False)
                        P.mm(psY[:, :], CT[1][s_][:, :], Sim[s_][:, :], start=False, stop=(k == 3))
                        k += 1
                    P.stt(yv[:, :], uF[:, ch, tsl], dcol[:, ch:ch + 1], psY[:, :], ALU.mult, ALU.add)
                    P.act(zg[:, ch, :], yv[:, :], AF.Gelu)
                sob = so[tb % 2]
                for oc in range(2):
                    for kc in range(2):
                        P.mm(psG[0][:, :], gw[:, kc, oc * 128:(oc + 1) * 128], zg[:, kc, :], start=(kc == 0), stop=(kc == 1))
                    for kc in range(2):
                        P.mm(psG[1][:, :], gw[:, kc, (oc + 2) * 128:(oc + 3) * 128], zg[:, kc, :], start=(kc == 0), stop=(kc == 1))
                    P.act(sig[:, :], psG[1][:, :], AF.Sigmoid, bias=gb[:, oc + 2:oc + 3])
                    P.stt(sob[:, oc, :], psG[0][:, :], gb[:, oc:oc + 1], sig[:, :], ALU.add, ALU.mult)
                P.dma('sp', s5v[:, :, tsl], sob[:, :, :])

    def gdn_prep_stage(l, PT, GQ):
        w = W[l]
        with P.stage():
            ptv = PT.re('(c p) t -> p c t', p=128)
            gqv = GQ.re('(c p) t -> p c t', p=128)
            convw = P.sbuf([128, 12, 4], F32)
            P.dma('sp', convw[:, :, :], w['convT'][:, :, :])
            xin = [P.sbuf([128, L], BF16) for _ in range(2)]
            outb = [P.sbuf([128, L], BF16) for _ in range(2)]
            acc = P.sbuf([128, L], F32)
            qs = P.sbuf([128, L], F32)
            sq = P.sbuf([128, L], BF16)
            rn = [P.sbuf([128, 512], F32) for _ in range(2)]
            pss = [P.psum([128, 512]) for _ in range(2)]
            for cc in range(12):
                x = xin[cc % 2]
                ob = outb[cc % 2]
                P.dma('sp', x[:, :], ptv[:, cc, :])
                P.ts('dve', acc[:, :], x[:, :], convw[:, cc, 3:4], ALU.mult)
                for j in (2, 1, 0):
                    sh = 3 - j
                    P.stt(acc[:, sh:L], x[:, 0:L - sh], convw[:, cc, j:j + 1], acc[:, sh:L], ALU.mult, ALU.add)
                if cc >= 8:
                    P.act(ob[:, :], acc[:, :], AF.Silu)
                else:
                    P.act(qs[:, :], acc[:, :], AF.Silu)
                    P.act(sq[:, :], qs[:, :], AF.Square)
                    for tb in range(8):
                        tsl = slice(tb * 512, (tb + 1) * 512)
                        ps = pss[tb % 2]
                        r_ = rn[tb % 2]
                        P.mm(ps[:, :], onesb[:, :], sq[:, tsl])
                        P.act(r_[:, :], ps[:, :], AF.Sqrt, bias=1e-6)
                        P.recip(r_[:, :], r_[:, :])
                        if cc < 4:
                            P.stt(ob[:, tsl], qs[:, tsl], float(128 ** -0.5), r_[:, :], ALU.mult, ALU.mult)
                        else:
                            P.tt('dve', ob[:, tsl], qs[:, tsl], r_[:, :], ALU.mult)
                P.dma('sp', gqv[:, cc, :], ob[:, :])

    def gdn_stage(l, PT, GQ, GD, baTM):
        w = W[l]
        with P.stage():
            gqv = GQ.re('(c p) t -> p c t', p=128)
            ptv = PT.re('(c p) t -> p c t', p=128)
            gdv = GD.re('(c p) t -> p c t', p=128)
            qkv = P.sbuf([128, 12, L], BF16)
            qkvh = [qkv.alias() for _ in range(12)]
            for cc in range(12):
                P.dma('sp', View(qkvh[cc], qkv.t[:, cc, :]), gqv[:, cc, :])
            gab = P.sbuf([128, 8], F32)
            gnw = P.sbuf([128, 1], F32)
            P.dma('sp', gab[:, :], w['gdn_ab'][:, :])
            P.dma('sp', gnw[:, :], w['gnw'][:, :])
            T3 = lambda: P.sbuf([128, NTILE, 4], F32)
            M = lambda: P.sbuf([128, 128], F32)
            bet, xg, g3 = T3(), T3(), T3()
            P.act(bet[:, :, :], baTM[:, :, 0:4], AF.Sigmoid)
            P.tt('dve', xg[:, :, :], baTM[:, :, 4:8], gab[:, 4:8].re('p (o h) -> p o h', o=1).bc([128, NTILE, 4]), ALU.add)
            P.act(xg[:, :, :], xg[:, :, :], AF.Exp)
            P.act(xg[:, :, :], xg[:, :, :], AF.Ln, bias=1.0)
            ea = P.sbuf([128, 4], F32)
            P.act(ea[:, :], gab[:, 0:4], AF.Exp)
            P.tt('dve', g3[:, :, :], xg[:, :, :], ea[:, :].re('p (o h) -> p o h', o=1).bc([128, NTILE, 4]), ALU.mult)
            P.ts('dve', g3[:, :, :], g3[:, :, :], -1.0, ALU.mult)
            f2 = lambda b: View(b, b.t[:, :, :].rearrange('p i h -> p (i h)'))
            gcol = f2(g3)
            betc = f2(bet)
            pbank = [P.psum([128, 512]) for _ in range(7)]
            psbb = P.psum([128, 1024], BF16)
            gc, gl, egc, edl, nbeta, bge, ng = M(), M(), M(), M(), M(), M(), M()
            P.mm(pbank[0][:, 0:128], cm[:, C_U2, :], gcol)
            P.cp('dve', gc[:, :], pbank[0][:, 0:128])
            P.mm(pbank[0][:, 128:256], cm[:, C_B2, :], gcol)
            P.cp('dve', gl[:, :], pbank[0][:, 128:256])
            P.act(egc[:, :], gc[:, :], AF.Exp)
            P.tt('dve', edl[:, :], gl[:, :], gc[:, :], ALU.subtract)
            P.act(edl[:, :], edl[:, :], AF.Exp)
            P.ts('dve', nbeta[:, :], betc, -1.0, ALU.mult)
            P.tt('dve', bge[:, :], betc, egc[:, :], ALU.mult)
            P.ts('dve', ng[:, :], gcol, -1.0, ALU.mult)
            edl0, edl1 = M(), M()
            P.ts('dve', edl0[:, :], edl[:, :], cv[:, 0:1], ALU.mult)
            P.ts('dve', edl1[:, :], edl[:, :], cv[:, 1:2], ALU.mult)
            gm = P.sbuf([128, 2, 128], F32)
            P.ts('dve', gm[:, 0, :], gcol, cv[:, 0:1], ALU.mult)
            P.ts('dve', gm[:, 1, :], gcol, cv[:, 1:2], ALU.mult)
            EGL = P.sbuf([128, 2, 128], F32)
            P.mm(pbank[1][:, 0:256], cm[:, C_ONES, :], View(gm, gm.t[:, :, :].rearrange('p a b -> p (a b)')))
            P.act(View(EGL, EGL.t[:, :, :].rearrange('p a b -> p (a b)')), pbank[1][:, 0:256], AF.Exp)
            H = []
            for h in range(4):
                d = {}
                for nm in ['GU', 'nG', 'Dm', 'Dn', 'dec', 'decT', 'N', 'pA', 'pB', 'pTA', 'pTB', 'tT', 'usb', 'S', 'osb', 'osq']:
                    d[nm] = P.sbuf([128, 128], F32)
                for nm in ['bgk', 'kd0', 'kd1', 'bv', 'TTb', 'wT', 'qkm', 'Sbf', 'vnew', 'on', 'zt', 'zs', 'og']:
                    d[nm] = P.sbuf([128, 128], BF16)
                d['ss'] = P.sbuf([128, 1], F32)
                d['rr'] = P.sbuf([128, 1], F32)
                d['ps'] = []
                for k in range(7):
                    idx = h * 7 + k
                    bk = pbank[idx // 4]
                    d['ps'].append(View(bk, bk.t[:, (idx % 4) * 128:(idx % 4 + 1) * 128]))
                d['pb'] = [View(psbb, psbb.t[:, (2 * h + k) * 128:(2 * h + k + 1) * 128]) for k in range(2)]
                P.memset('pool', d['S'][:, :], 0.0)
                P.memset('pool', d['Sbf'][:, :], 0.0)
                P.memset('pool', d['vnew'][:, :], 0.0)
                H.append(d)
            U2 = cm[:, C_U2, :]
            B2 = cm[:, C_B2, :]
            SL2 = cm[:, C_SL2, :]
            IDf = cm[:, C_ID, :]
            _lim = int(_os_env('GDN_STEPS') or 999)
            _go = lambda n: n <= _lim
            for i in range(int(_os_env('GDN_NT') or NTILE)):
                tsl = slice(i * 128, (i + 1) * 128)
                qTt = [View(qkvh[h], qkv.t[:, h, tsl]) for h in range(4)]
                kTt = [View(qkvh[4 + h], qkv.t[:, 4 + h, tsl]) for h in range(4)]
                vTt = [View(qkvh[8 + h], qkv.t[:, 8 + h, tsl]) for h in range(4)]
                col = lambda tab, h: tab[:, i * 4 + h:i * 4 + h + 1]
                for h in (range(4) if _go(1) else ()):
                    d = H[h]
                    _b = int(_os_env('GDN_S1') or 255)
                    if _b & 1:
                        P.tr(d['pb'][0], kTt[h], identb[:, :])
                    if _b & 2:
                        P.tr(d['pb'][1], vTt[h], identb[:, :])
                    if _b & 4:
                        P.act(d['bgk'][:, :], d['pb'][0], AF.Copy, scale=col(bge, h))
                    if _b & 8:
                        P.ts('dve', d['kd0'][:, :], d['pb'][0], col(edl0, h), ALU.mult)
                    if _b & 16:
                        P.ts('dve', d['kd1'][:, :], d['pb'][0], col(edl1, h), ALU.mult)
                    if _b & 32:
                        P.act(d['bv'][:, :], d['pb'][1], AF.Copy, scale=col(betc, h))
                    if _b & 64:
                        P.ts('pool', d['GU'][:, :], U2, col(gcol, h), ALU.mult, 1.0, ALU.mult)
                    if _b & 128:
                        P.ts('pool', d['nG'][:, :], cm[:, C_ONES, :], col(ng, h), ALU.mult, 1.0, ALU.mult)
                for h in (range(4) if _go(2) else ()):
                    d = H[h]
                    P.mm(d['ps'][0], d['GU'][:, :], B2, start=True, stop=False)
                    P.mm(d['ps'][0], d['nG'][:, :], U2, start=False, stop=True)
                    P.mm(d['ps'][1], kTt[h], kTt[h])
                    P.mm(d['ps'][4], kTt[h], qTt[h])
                for h in (range(4) if _go(3) else ()):
                    d = H[h]
                    P.ts('dve', d['Dm'][:, :], d['ps'][0], 0.0, ALU.min)
                    P.ts('dve', d['Dn'][:, :], d['ps'][0], -1.0, ALU.mult, 0.0, ALU.min)
                    P.act(d['dec'][:, :], d['Dm'][:, :], AF.Exp)
                    P.act(d['decT'][:, :], d['Dn'][:, :], AF.Exp)
                    P.tt('pool', d['dec'][:, :], d['dec'][:, :], SL2, ALU.mult)
                    P.tt('pool', d['decT'][:, :], d['decT'][:, :], U2, ALU.mult)
                    P.stt(d['N'][:, :], d['ps'][1], col(nbeta, h), d['dec'][:, :], ALU.mult, ALU.mult)
                    P.tt('dve', d['qkm'][:, :], d['ps'][4], d['decT'][:, :], ALU.mult)
                for h in (range(4) if _go(4) else ()):
                    d = H[h]
                    P.tr(d['ps'][2], d['N'][:, :], IDf)
                for h in (range(4) if _go(5) else ()):
                    d = H[h]
                    P.cp('act', d['pTA'][:, :], d['ps'][2])
                    P.tt('dve', d['tT'][:, :], d['ps'][2], IDf, ALU.add)
                for h in (range(4) if _go(6) else ()):
                    H[h]['p'], H[h]['pT'], H[h]['pn'], H[h]['pTn'] = H[h]['N'], H[h]['pTA'], H[h]['pA'], H[h]['pTB']
                for it in range(5):
                    for h in (range(4) if _go(7) else ()):
                        d = H[h]
                        P.mm(d['ps'][2], d['pT'][:, :], d['p'][:, :])
                        if it < 4:
                            P.mm(d['ps'][3], d['p'][:, :], d['pT'][:, :])
                    for h in (range(4) if _go(8) else ()):
                        d = H[h]
                        P.cp('act', d['pn'][:, :], d['ps'][2])
                        if it < 4:
                            P.cp('dve', d['pTn'][:, :], d['ps'][3])
                    for h in (range(4) if _go(9) else ()):
                        d = H[h]
                        P.mm(d['ps'][4], d['pn'][:, :], d['tT'][:, :])
                    for h in (range(4) if _go(10) else ()):
                        d = H[h]
                        P.tt('dve', d['tT'][:, :], d['tT'][:, :], d['ps'][4], ALU.add)
                        oldp, oldpT = d['p'], d['pT']
                        d['p'], d['pT'] = d['pn'], d['pTn']
                        d['pn'] = d['pB'] if d['p'] is d['pA'] else d['pA']
                        d['pTn'] = d['pTA'] if d['pT'] is d['pTB'] else d['pTB']
                for h in (range(4) if _go(11) else ()):
                    d = H[h]
                    P.cp('act', d['TTb'][:, :], d['tT'][:, :])
                for h in (range(4) if _go(12) else ()):
                    d = H[h]
                    P.mm(d['ps'][0], d['TTb'][:, :], d['bv'][:, :])
                    P.mm(d['ps'][1], d['bgk'][:, :], d['TTb'][:, :])
                for h in (range(4) if _go(13) else ()):
                    d = H[h]
                    P.cp('act', d['usb'][:, :], d['ps'][0])
                    P.cp('dve', d['wT'][:, :], d['ps'][1])
                for c in range(2):
                    r = slice(c * 64, (c + 1) * 64)
                    for h in (range(4) if _go(14) else ()):
                        d = H[h]
                        P.mm(d['ps'][5], d['wT'][:, :], d['Sbf'][:, :])
                        P.mm(d['ps'][6], qTt[h], d['Sbf'][:, :])
                    for h in (range(4) if _go(15) else ()):
                        d = H[h]
                        P.tt('dve', d['vnew'][r, :], d['usb'][r, :], d['ps'][5][r, :], ALU.subtract)
                        P.act(d['osb'][r, :], d['ps'][6][r, :], AF.Copy, scale=View(egc, egc.t[r, i * 4 + h:i * 4 + h + 1]))
                    for h in (range(4) if _go(16) else ()):
                        d = H[h]
                        P.mm(d['ps'][0], d['qkm'][:, :], d['vnew'][:, :])
                        P.mm(d['ps'][1], d['kd%d' % c][:, :], d['vnew'][:, :])
                    for h in (range(4) if _go(17) else ()):
                        d = H[h]
                        P.tt('dve', d['osb'][r, :], d['osb'][r, :], d['ps'][0][r, :], ALU.add)
                        P.stt(d['S'][:, :], d['S'][:, :], EGL[:, c, i * 4 + h:i * 4 + h + 1], d['ps'][1], ALU.mult, ALU.add)
                        P.cp('act', d['Sbf'][:, :], d['S'][:, :])
                for h in (range(4) if _go(18) else ()):
                    d = H[h]
                    P.dma('sp', d['zt'][:, :], ptv[:, 12 + h, tsl])
                    P.act(d['osq'][:, :], d['osb'][:, :], AF.Square, accum=d['ss'][:, :])
                    P.act(d['rr'][:, :], d['ss'][:, :], AF.Sqrt, bias=1e-6, scale=1.0 / 128.0)
                    P.recip(d['rr'][:, :], d['rr'][:, :])
                    P.ts('dve', d['on'][:, :], d['osb'][:, :], d['rr'][:, 0:1], ALU.mult)
                    P.act(d['zs'][:, :], d['zt'][:, :], AF.Silu)
                for h in (range(4) if _go(19) else ()):
                    d = H[h]
                    P.tr(d['pb'][0], d['on'][:, :], identb[:, :])
                for h in (range(4) if _go(20) else ()):
                    d = H[h]
                    P.stt(d['og'][:, :], d['pb'][0], gnw[:, 0:1], d['zs'][:, :], ALU.mult, ALU.mult)
                    P.dma('sp', gdv[:, h, tsl], d['og'][:, :])

    def moba_stage(l, PT, MO):
        with P.stage():
            ptv = PT.re('(c p) t -> p c t', p=128)
            qb_ = P.sbuf([128, 2, L], BF16)
            kb_ = P.sbuf([128, 2, L], BF16)
            vb_ = P.sbuf([128, 2, L], BF16)
            P.dma('sp', qb_[:, :, :], ptv[:, 18:20, :])
            P.dma('sp', kb_[:, :, :], ptv[:, 20:22, :])
            P.dma('sp', vb_[:, :, :], ptv[:, 22:24, :])
            cmf = P.sbuf([128, 2, 256], F32)
            cmk = P.sbuf([128, 2, 256], BF16)
            P.dma('sp', cmf[:, :, :], cmaskD[:, :, :])
            P.cp('dve', cmk[:, :, :], cmf[:, :, :])
            stf = P.sbuf([128, 16, 128], F32)
            selT = P.sbuf([128, 16, 128], BF16)
            qz = P.sbuf([128, 4, L], BF16)
            for h in range(4):
                P.ts('pool' if h % 2 else 'dve', qz[:, h, :], qb_[:, h // 2, :], cv[:, (h % 2):(h % 2) + 1], ALU.mult)
            P.dma('sp', stf[:, :, :], selTD[:, :, :])
            P.cp('dve', selT[:, :, :], stf[:, :, :])
            psb = P.psum([128, 1024], BF16)
            psbs = [psb, psb]
            Vtm = P.sbuf([128, NTILE, 260], BF16)
            P.memset('pool', Vtm[:, :, :], 1.0)
            k = 0
            for i in range(NTILE):
                for c in range(2):
                    pb = psbs[k % 2]
                    po = (k % 2) * 128
                    k += 1
                    P.tr(View(pb, psb.t[:, po:po + 128]), vb_[:, c, i * 128:(i + 1) * 128], identb[:, :])
                    for hh in range(2):
                        h = 2 * c + hh
                        P.cp('act' if hh else 'dve', Vtm[:, i, h * 65:h * 65 + 64],
                             View(pb, psb.t[:, po + hh * 64:po + (hh + 1) * 64]))
            kmT = P.sbuf([128, 2, 16], F32)
            for c in range(2):
                P.op('dve', lambda e, c=c: e.tensor_reduce(out=kmT.t[:, c, :], in_=kb_.t[:, c, :].rearrange('p (n s) -> p n s', s=256),
                                                           axis=AX.X, op=ALU.add), reads=[kb_], writes=[kmT])
            P.ts('dve', kmT[:, :, :], kmT[:, :, :], 1.0 / 256.0, ALU.mult)
            psmisc = P.psum([128, 512])
            psg = psmisc
            pstf = psmisc
            psS = P.psum([128, 512])
            psST = [P.psum([128, 512]) for _ in range(2)]
            psO = [P.psum([128, 512]) for _ in range(2)]
            qf = P.sbuf([128, 4, 128], F32)
            gpad = P.sbuf([128, 4, 16], F32)
            top8 = P.sbuf([128, 4, 8], F32)
            selm = P.sbuf([128, 4, 16], F32)
            bias = P.sbuf([128, 4, 16], F32)
            mx = P.sbuf([128, 4, 8], F32)
            mrow = P.sbuf([128, 4], F32)
            biasT = [P.sbuf([128, 4, 256], BF16) for _ in range(2)]
            for b_ in biasT:
                P.memset('pool', b_[:, :, :], 0.0)
            PTs = [P.sbuf([128, 256], BF16) for _ in range(2)]
            rden = P.sbuf([128, 1], F32)
            Otm = P.sbuf([128, NTILE, 256], BF16)
            kk = 0
            for qbi in range(16):
                bT = biasT[qbi % 2]
                for half in range(2):
                    i = 2 * qbi + half
                    tsl = slice(i * 128, (i + 1) * 128)
                    P.cp('pool', qf[:, :, :], qz[:, :, tsl])
                    P.memset('pool', selm[:, :, :], 0.0)
                    if qbi > 0:
                        for h in range(4):
                            c, r0 = h // 2, (h % 2) * 64
                            P.mm(View(psg, psmisc.t[:, h * 16:(h + 1) * 16]), qf[:, h, :], kmT[:, c, :])
                        if qbi <= 3:
                            P.memset('pool', selm[:, :, 0:qbi], 1.0)
                        else:
                            P.memset('dve', gpad[:, :, :], -1e30)
                            P.cp('dve', gpad[:, :, 0:qbi],
                                 View(psg, psmisc.t[:, 0:64].rearrange('p (h n) -> p h n', n=16)[:, :, 0:qbi]))
                            for h in range(4):
                                P.op('dve', lambda e, h=h: e.max(out=top8.t[:, h, :], in_=gpad.t[:, h, :]),
                                     reads=[gpad], writes=[top8])
                                P.ts('dve', selm[:, h, 0:qbi], gpad[:, h, 0:qbi], top8[:, h, 2:3], ALU.is_ge)
                    P.memset('pool', selm[:, :, qbi:qbi + 1], 1.0)
                    nj = (qbi + 2) // 2
                    for h in range(4):
                        c, r0 = h // 2, (h % 2) * 64
                        for j in range(nj):
                            P.mm(psS[:, :], qz[:, h, tsl], kb_[:, c, j * 512:(j + 1) * 512])
                            P.op('dve', lambda e, h=h, j=j: e.reduce_max(out=mx.t[:, h, j:j + 1], in_=psS.t[:, :], axis=AX.X),
                                 reads=[psS], writes=[mx])
                        P.op('dve', lambda e, h=h, nj=nj: e.reduce_max(out=mrow.t[:, h:h + 1], in_=mx.t[:, h, 0:nj], axis=AX.X),
                             reads=[mx], writes=[mrow])
                    P.ts('dve', bias[:, :, :], selm[:, :, :], BIGRAW, ALU.mult, -BIGRAW, ALU.add)
                    P.tt('dve', bias[:, :, :], bias[:, :, :], mrow[:, :].re('p (h o) -> p h o', o=1).bc([128, 4, 16]), ALU.subtract)
                    for h in range(4):
                        P.tr(View(pstf, psmisc.t[0:16, 128:256]), bias[:, h, :], cm[:, C_ID, :])
                        P.cp('act', bT[0:16, h, half * 128:(half + 1) * 128], View(pstf, psmisc.t[0:16, 128:256]))
                qsl = slice(qbi * 256, (qbi + 1) * 256)
                for h in range(4):
                    c, r0 = h // 2, (h % 2) * 64
                    nst = 2 * (qbi + 1)
                    for st_ in range(nst):
                        kb, sc = st_ // 2, st_ % 2
                        ps = psST[kk % 2]
                        pt = PTs[kk % 2]
                        kk += 1
                        own = (kb == qbi)
                        P.mm(ps[:, 0:256], kb_[:, c, st_ * 128:(st_ + 1) * 128], qz[:, h, qsl],
                             start=True, stop=False)
                        P.mm(ps[:, 0:256], selT[:, kb, :], bT[:, h, :], start=False, stop=(not own))
                        if own:
                            P.mm(ps[:, 0:256], identb[:, :], cmk[:, sc, :], start=False, stop=True)
                        P.act(pt[:, :], ps[:, 0:256], AF.Exp, scale=0.125)
                        for th in range(2):
                            P.mm(psO[th][:, 0:65], pt[:, th * 128:(th + 1) * 128], Vtm[:, st_, h * 65:(h + 1) * 65],
                                 start=(st_ == 0), stop=(st_ == nst - 1))
                    for th in range(2):
                        P.recip(rden[:, :], psO[th][:, 64:65])
                        P.ts('dve', Otm[:, 2 * qbi + th, h * 64:(h + 1) * 64], psO[th][:, 0:64], rden[:, 0:1], ALU.mult)
            MOsb = P.sbuf([128, 2, L], BF16)
            k = 0
            for i in range(NTILE):
                for c in range(2):
                    pb = psbs[k % 2]
                    po = (k % 2) * 128
                    k += 1
                    P.tr(View(pb, psb.t[:, po:po + 128]), Otm[:, i, c * 128:(c + 1) * 128], identb[:, :])
                    P.cp('act' if c else 'dve', MOsb[:, c, i * 128:(i + 1) * 128], View(pb, psb.t[:, po:po + 128]))
            P.dma('sp', MO.re('(c p) t -> p c t', p=128), MOsb[:, :, :])

    def peer_cast_stage(l, UTb, Vb):
        w = W[l]
        with P.stage():
            stg = [P.sbuf([128, 8, 512], F32) for _ in range(2)]
            ob = [P.sbuf([128, 8, 512], BF16) for _ in range(2)]
            uv = w['uT'].re('(kc p) e -> p kc e', p=128)
            uo = UTb.re('(kc p) e -> p kc e', p=128)
            vv = w['vtab'].re('(ec p) d -> p ec d', p=128)
            vo = Vb.re('(ec p) d -> p ec d', p=128)
            k = 0
            engs = ['pool', 'act', 'dve']
            for pc in range(32):
                st, o_ = stg[k % 2], ob[k % 2]
                P.dma('sp', st[:, :, :], uv[:, :, pc * 512:(pc + 1) * 512])
                P.cp(engs[k % 3], o_[:, :, :], st[:, :, :])
                P.dma('sp', uo[:, :, pc * 512:(pc + 1) * 512], o_[:, :, :])
                k += 1
            for pc in range(32):
                st, o_ = stg[k % 2], ob[k % 2]
                sv = View(st, st.t[:, :, :].rearrange('p a (b c) -> p (a b) c', b=1)) if False else None
                stv = View(st, st.t[:, :, :].rearrange('p a b -> p (a b)').rearrange('p (a b) -> p a b', a=4))
                obv = View(o_, o_.t[:, :, :].rearrange('p a b -> p (a b)').rearrange('p (a b) -> p a b', a=4))
                P.dma('sp', stv, vv[:, pc * 4:(pc + 1) * 4, :])
                P.cp(engs[k % 3], o_[:, :, :], st[:, :, :])
                P.dma('sp', vo[:, pc * 4:(pc + 1) * 4, :], obv)
                k += 1

    def peer_prep_stage(l, Xin, H2, QS):
        w = W[l]
        with P.stage():
            hT = P.sbuf([128, 8, L], BF16)
            hblk = [hT.alias() for _ in range(8)]
            norm_mod(Xin, A2[l], mod[l][:, 24:32], hblk, hT)
            h2v = H2.re('(kc p) t -> p kc t', p=128)
            for tb in range(8):
                P.dma('sp', h2v[:, :, tb * 512:(tb + 1) * 512], View(hblk[tb], hT.t[:, :, tb * 512:(tb + 1) * 512]))
            wstg = [P.sbuf([128, 8, 512], F32) for _ in range(2)]
            wblk = [P.sbuf([128, 8, 512], BF16) for _ in range(2)]
            ots = [P.sbuf([128, 4, 512], F32) for _ in range(2)]
            pss = [P.psum([128, 512]) for _ in range(4)]
            wv = w['wq'].re('(kc p) j -> p kc j', p=128)
            qsv = QS.re('(c p) t -> p c t', p=128)
            k = 0
            for cb in range(4):
                wb = wblk[cb % 2]
                P.dma('sp', wstg[cb % 2][:, :, :], wv[:, :, cb * 512:(cb + 1) * 512])
                P.cp('pool', wb[:, :, :], wstg[cb % 2][:, :, :])
                for tb in range(8):
                    ot = ots[(cb * 8 + tb) % 2]
                    for j in range(4):
                        ps = pss[k % 4]
                        k += 1
                        for kc in range(8):
                            P.mm(ps[:, :], wb[:, kc, j * 128:(j + 1) * 128],
                                 View(hblk[tb], hT.t[:, kc, tb * 512:(tb + 1) * 512]), start=(kc == 0), stop=(kc == 7))
                        P.cp('act' if k % 2 else 'dve', ot[:, j, :], ps[:, :])
                    P.dma('sp', qsv[:, cb * 4:(cb + 1) * 4, tb * 512:(tb + 1) * 512], ot[:, :, :])

    def peer_stage(l, Xin, Xout, H2, QS, UTb, Vb, ntb=16):
        w = W[l]
        EBS = 1024
        NI1 = EBS // 128
        NEB = 16384 // EBS
        with P.stage():
            k1T = P.sbuf([128, 8, 128], F32)
            k2T = P.sbuf([128, 8, 128], F32)
            P.dma('sp', k1T[:, :, :], w['k1T'].re('h d n -> d h n'))
            P.dma('sp', k2T[:, :, :], w['k2T'].re('h d n -> d h n'))
            h2v = H2.re('(kc p) t -> p kc t', p=128)
            qsv = QS.re('(c p) t -> p c t', p=128)
            xv = Xin.re('(kc p) t -> p kc t', p=128)
            ov = Xout.re('(kc p) t -> p kc t', p=128)
            utv = UTb.re('(kc p) e -> p kc e', p=128)
            vbv = Vb.re('(ec p) d -> p ec d', p=128)
            hs = [P.sbuf([128, 8, 256], BF16) for _ in range(2)]
            qT = [P.sbuf([128, 16, 256], F32) for _ in range(2)]
            xts = [P.sbuf([128, 8, 256], F32) for _ in range(2)]
            ot = P.sbuf([128, 8, 256], F32)
            UT = [P.sbuf([128, 8, EBS], BF16) for _ in range(2)]
            VB = [P.sbuf([128, NI1, D], BF16) for _ in range(2)]
            E1 = P.sbuf([128, 2, 8, 128], F32)
            E2 = P.sbuf([128, 2, 8, 128], F32)
            TH = P.sbuf([128, 2, 8], F32)
            sc = [P.sbuf([128, 256], F32) for _ in range(2)]
            scr = P.sbuf([128, 256], F32)
            v12 = P.sbuf([128, 2, 16], F32)
            cand = P.sbuf([128, 16, 16], F32)
            cscr = P.sbuf([128, 256], F32)
            t16 = P.sbuf([128, 16], F32)
            junk = P.sbuf([128, 16], F32)
            e1t = P.sbuf([128, 128], F32)
            sm1 = lambda: P.sbuf([128, 1], F32)
            nm1, nm2, nm, Z, thr = sm1(), sm1(), sm1(), sm1(), sm1()
            Wsum = [P.sbuf([128, NI1, 128], F32) for _ in range(2)]
            wtmp = [P.sbuf([128, NI1, 128], F32) for _ in range(2)]
            wtmp2 = P.sbuf([128, NI1, 128], F32)
            gel = [P.sbuf([128, 512], F32) for _ in range(2)]
            Wg = [P.sbuf([128, 512], BF16) for _ in range(2)]
            WgT = [P.sbuf([128, 4, 128], BF16) for _ in range(2)]
            Ysb = P.sbuf([128, D], F32)
            psY = [[P.psum([128, 512]) for _ in range(2)] for _ in range(2)]
            psA = [P.psum([128, 512]) for _ in range(2)]
            psT = P.psum([128, 1024], BF16)
            psM = P.psum([128, 512])
            flat = lambda b: View(b, b.t[:, :, :].rearrange('p a b -> p (a b)'))
            ka = 0
            for tb in range(ntb):
                tsl = slice(tb * 256, (tb + 1) * 256)
                h_, q_, xt = hs[tb % 2], qT[tb % 2], xts[tb % 2]
                P.dma('sp', h_[:, :, :], h2v[:, :, tsl])
                P.dma('sp', q_[:, :, :], qsv[:, :, tsl])
                P.dma('sp', xt[:, :, :], xv[:, :, tsl])
                for tile in range(2):
                    cs_ = slice(tile * 128, (tile + 1) * 128)
                    for h in range(8):
                        s_ = sc[h % 2]
                        P.mm(psM[:, 0:128], q_[:, 2 * h, cs_], k1T[:, h, :])
                        P.mm(psM[:, 128:256], q_[:, 2 * h + 1, cs_], k2T[:, h, :])
                        P.cp('act', s_[:, :], psM[:, 0:256])
                        for hf in range(2):
                            o = hf * 128
                            P.op('dve', lambda e, hf=hf, o=o, s_=s_: e.max(out=v12.t[:, hf, 0:8], in_=s_.t[:, o:o + 128]),
                                 reads=[s_], writes=[v12])
                            P.op('dve', lambda e, hf=hf, o=o, s_=s_: e.match_replace(
                                out=scr.t[:, o:o + 128], in_to_replace=v12.t[:, hf, 0:8], in_values=s_.t[:, o:o + 128],
                                imm_value=-1e30), reads=[s_, v12], writes=[scr])
                            P.op('dve', lambda e, hf=hf, o=o: e.max(out=v12.t[:, hf, 8:16], in_=scr.t[:, o:o + 128]),
                                 reads=[scr], writes=[v12])
                        P.tt('dve', cand[:, :, :], v12[:, 0, :].re('p (a o) -> p a o', o=1).bc([128, 16, 16]),
                             v12[:, 1, :].re('p (o b) -> p o b', o=1).bc([128, 16, 16]), ALU.add)
                        P.op('dve', lambda e: e.max(out=t16.t[:, 0:8], in_=cand.t[:, :, :].rearrange('p a b -> p (a b)')),
                             reads=[cand], writes=[t16])
                        P.op('dve', lambda e: e.match_replace(out=cscr.t[:, :], in_to_replace=t16.t[:, 0:8],
                                                              in_values=cand.t[:, :, :].rearrange('p a b -> p (a b)'),
                                                              imm_value=-1e30), reads=[cand, t16], writes=[cscr])
                        P.op('dve', lambda e: e.max(out=t16.t[:, 8:16], in_=cscr.t[:, :]), reads=[cscr], writes=[t16])
                        P.ts('dve', nm1[:, :], v12[:, 0, 0:1], -1.0, ALU.mult)
                        P.ts('dve', nm2[:, :], v12[:, 1, 0:1], -1.0, ALU.mult)
                        P.ts('dve', nm[:, :], t16[:, 0:1], -1.0, ALU.mult)
                        P.act(junk[:, :], t16[:, :], AF.Exp, bias=nm[:, 0:1], accum=Z[:, :])
                        P.recip(Z[:, :], Z[:, :])
                        P.act(E2[:, tile, h, :], s_[:, 128:256], AF.Exp, bias=nm2[:, 0:1])
                        P.act(e1t[:, :], s_[:, 0:128], AF.Exp, bias=nm1[:, 0:1])
                        P.ts('dve', E1[:, tile, h, :], e1t[:, :], Z[:, 0:1], ALU.mult)
                        P.ts('dve', nm[:, :], nm[:, :], -1e-3, ALU.add)
                        P.act(thr[:, :], t16[:, 15:16], AF.Exp, bias=nm[:, 0:1])
                        P.tt('dve', TH[:, tile, h:h + 1], thr[:, :], Z[:, :], ALU.mult)
                for eb in range(NEB):
                    ut, vb = UT[eb % 2], VB[eb % 2]
                    P.dma('sp', ut[:, :, :], utv[:, :, eb * EBS:(eb + 1) * EBS])
                    P.dma('sp', vb[:, :, :], vbv[:, eb * NI1:(eb + 1) * NI1, :])
                    for tile in range(2):
                        cs_ = slice(tile * 128, (tile + 1) * 128)
                        ws = Wsum[tile]
                        for h in range(8):
                            wt = wtmp[h % 2]
                            P.tt('pool', wt[:, :, :],
                                 E1[:, tile, h, eb * NI1:(eb + 1) * NI1].re('p (a o) -> p a o', o=1).bc([128, NI1, 128]),
                                 E2[:, tile, h, :].re('p (o b) -> p o b', o=1).bc([128, NI1, 128]), ALU.mult)
                            if h == 0:
                                P.stt(flat(ws), flat(wt), TH[:, tile, h:h + 1], flat(wt), ALU.is_ge, ALU.mult)
                            else:
                                P.stt(flat(wtmp2), flat(wt), TH[:, tile, h:h + 1], flat(wt), ALU.is_ge, ALU.mult)
                                P.tt('dve', flat(ws), flat(ws), flat(wtmp2), ALU.add)
                        for sub in range(EBS // 512):
                            pa = psA[ka % 2]
                            g_, wg_, wgt_ = gel[ka % 2], Wg[ka % 2], WgT[ka % 2]
                            ka += 1
                            for kc in range(8):
                                P.mm(pa[:, :], h_[:, kc, cs_], ut[:, kc, sub * 512:(sub + 1) * 512],
                                     start=(kc == 0), stop=(kc == 7))
                            P.act(g_[:, :], pa[:, :], AF.Gelu)
                            P.tt('dve', wg_[:, :], g_[:, :], flat(ws)[:, sub * 512:(sub + 1) * 512], ALU.mult)
                            for j in range(4):
                                P.tr(psT[:, j * 128:(j + 1) * 128], wg_[:, j * 128:(j + 1) * 128], identb[:, :])
                            P.cp('act', flat(wgt_), psT[:, 0:512])
                            for j in range(4):
                                ec = sub * 4 + j
                                first = (eb == 0 and sub == 0 and j == 0)
                                last = (eb == NEB - 1 and sub == EBS // 512 - 1 and j == 3)
                                for half in range(2):
                                    P.mm(psY[tile][half][:, :], wgt_[:, j, :], vb[:, ec, half * 512:(half + 1) * 512],
                                         start=first, stop=last)
                for tile in range(2):
                    cs_ = slice(tile * 128, (tile + 1) * 128)
                    P.cp('act', Ysb[:, 0:512], psY[tile][0][:, :])
                    P.cp('dve', Ysb[:, 512:1024], psY[tile][1][:, :])
                    for dc in range(8):
                        pa = psA[dc % 2]
                        P.tr(pa[:, 0:128], Ysb[:, dc * 128:(dc + 1) * 128], cm[:, C_ID, :])
                        P.stt(ot[:, dc, cs_], pa[:, 0:128], mod[l][:, 40 + dc:41 + dc], xt[:, dc, cs_], ALU.mult, ALU.add)
                P.dma('sp', ov[:, :, tsl], ot[:, :, :])

    def merge_stage(l, PT, GD, S5O, MO, Xin, Xout, branches):
        w = W[l]
        TBM = 256
        with P.stage():
            wstg = P.sbuf([128, 8, D], F32)

            def loadw(src, nk):
                f = P.sbuf([128, nk, D], BF16)
                P.dma('sp', wstg[:, 0:nk, :], src.re('(kc p) j -> p kc j', p=128))
                P.cp('pool', f[:, :, :], wstg[:, 0:nk, :])
                return f
            wa, wb_, wc, wo = loadw(w['wa'], 4), loadw(w['wb'], 2), loadw(w['wc'], 2), loadw(w['wout'], 8)
            ptv = PT.re('(c p) t -> p c t', p=128)
            xv = Xin.re('(kc p) t -> p kc t', p=128)
            ov = Xout.re('(kc p) t -> p kc t', p=128)
            srcs = [(GD, 4, wa), (S5O, 2, wb_), (MO, 2, wc)]
            bts = [[P.sbuf([128, nk, TBM], BF16) for _ in range(2)] for (_, nk, _) in srcs]
            gts = [P.sbuf([128, 24, TBM], BF16) for _ in range(2)]
            xts = [P.sbuf([128, 8, TBM], F32) for _ in range(2)]
            ots = [P.sbuf([128, 8, TBM], F32) for _ in range(2)]
            mg = P.sbuf([128, 8, TBM], BF16)
            acc = P.sbuf([128, TBM], F32)
            tmp = P.sbuf([128, TBM], F32)
            pss = [P.psum([128, 512]) for _ in range(6)]
            k = 0
            for tb in range(L // TBM):
                tsl = slice(tb * TBM, (tb + 1) * TBM)
                for bi, (src, nk, _) in enumerate(srcs):
                    if bi in branches:
                        P.dma('sp', bts[bi][tb % 2][:, :, :], src.re('(c p) t -> p c t', p=128)[:, :, tsl])
                gt = gts[tb % 2]
                P.dma('sp', gt[:, :, :], ptv[:, 24:48, tsl])
                xt = xts[tb % 2]
                P.dma('sp', xt[:, :, :], xv[:, :, tsl])
                for dc in range(8):
                    first = True
                    for bi, (src, nk, wt) in enumerate(srcs):
                        if bi not in branches:
                            continue
                        ps = pss[k % 6]
                        k += 1
                        for kc in range(nk):
                            P.mm(ps[:, 0:TBM], wt[:, kc, dc * 128:(dc + 1) * 128], bts[bi][tb % 2][:, kc, :],
                                 start=(kc == 0), stop=(kc == nk - 1))
                        if first:
                            P.tt('dve', acc[:, :], ps[:, 0:TBM], gt[:, bi * 8 + dc, :], ALU.mult)
                            first = False
                        else:
                            P.tt('dve', tmp[:, :], ps[:, 0:TBM], gt[:, bi * 8 + dc, :], ALU.mult)
                            P.tt('pool', acc[:, :], acc[:, :], tmp[:, :], ALU.add)
                    P.cp('act', mg[:, dc, :], acc[:, :])
                ot = ots[tb % 2]
                for oc in range(8):
                    ps = pss[k % 6]
                    k += 1
                    for kc in range(8):
                        P.mm(ps[:, 0:TBM], wo[:, kc, oc * 128:(oc + 1) * 128], mg[:, kc, :], start=(kc == 0), stop=(kc == 7))
                    P.stt(ot[:, oc, :], ps[:, 0:TBM], mod[l][:, 16 + oc:17 + oc], xt[:, oc, :], ALU.mult, ALU.add)
                P.dma('sp', ov[:, :, tsl], ot[:, :, :])

    if peer_only:
        Xc = ein('XMin', [D, L])
        UTb = P.dram('UTb0', [D, 16384], BF16)
        Vb = P.dram('Vb0', [16384, D], BF16)
        H2 = P.dram('H2_0', [D, L], BF16)
        QS = dbg_dram('QS0', [2048, L], F32)
        Xo = dbg_dram('XO0', [D, L], F32)
        peer_cast_stage(0, UTb, Vb)
        peer_prep_stage(0, Xc, H2, QS)
        peer_stage(0, Xc, Xo, H2, QS, UTb, Vb, ntb=peer_ntb)
        P.close()
        return nc, dbg_out
    Xcur = xT
    for l in range(2):
        if upto < 1:
            break
        PT = dbg_dram('PT%d' % l, [48 * 128, L], BF16)
        baTM = P.sbuf([128, NTILE, 8], F32, persist=True, name='baTM%d' % l)
        if fake_pt and l == 0:
            PT = ein('PTin', [48 * 128, L], BF16)
            baIn = ein('baTMin', [128, NTILE, 8])
            with P.stage():
                P.dma('sp', baTM[:, :, :], baIn[:, :, :])
        with (contextlib.nullcontext() if (fake_pt and l == 0) else P.stage()):
          if not (fake_pt and l == 0):
              hT = P.sbuf([128, 8, L], BF16)
              hblk = [hT.alias() for _ in range(8)]
              norm_mod(Xcur, A1[l], mod[l][:, 0:8], hblk, hT)
              if 'h1' in dbg and l == 0:
                  dh = dbg_dram('h1', [128, 8, L], BF16)
                  P.dma('sp', dh[:, :, :], View(hT, hT.t[:, :, :]))
              wblk = [P.sbuf([128, 8, 512], BF16) for _ in range(2)]
              wstg = [P.sbuf([128, 8, 512], F32) for _ in range(2)]
              ots = [P.sbuf([128, 4, 512], BF16) for _ in range(2)]
              pss = [P.psum([128, 512]) for _ in range(4)]
              wv = W[l]['w_main'].re('(kc p) j -> p kc j', p=128)
              ptv = PT.re('(c p) t -> p c t', p=128)
              k = 0
              for cb in range(12):
                  wb = wblk[cb % 2]
                  P.dma('sp', wstg[cb % 2][:, :, :], wv[:, :, cb * 512:(cb + 1) * 512])
                  P.cp('pool', wb[:, :, :], wstg[cb % 2][:, :, :])
                  for tb in range(8):
                      ot = ots[(cb * 8 + tb) % 2]
                      for j in range(4):
                          ps = pss[k % 4]
                          k += 1
                          for kc in range(8):
                              P.mm(ps[:, :], wb[:, kc, j * 128:(j + 1) * 128],
                                   View(hblk[tb], hT.t[:, kc, tb * 512:(tb + 1) * 512]),
                                   start=(kc == 0), stop=(kc == 7))
                          chunk = cb * 4 + j
                          if chunk >= 24:
                              P.act(ot[:, j, :], ps[:, :], AF.Sigmoid)
                          elif k % 2 == 0:
                              P.cp('act', ot[:, j, :], ps[:, :])
                          else:
                              P.cp('dve', ot[:, j, :], ps[:, :])
                      P.dma('sp', ptv[:, cb * 4:(cb + 1) * 4, tb * 512:(tb + 1) * 512], ot[:, :, :])
              wbaf = P.sbuf([128, 8, 8], F32)
              wba = P.sbuf([128, 8, 8], BF16)
              P.dma('sp', wbaf[:, :, :], W[l]['w_ba'].re('(kc p) j -> p kc j', p=128))
              P.cp('dve', wba[:, :, :], wbaf[:, :, :])
              for i in range(NTILE):
                  ps = pss[i % 4]
                  for kc in range(8):
                      P.mm(ps[:, 0:8], View(hblk[i // 4], hT.t[:, kc, i * 128:(i + 1) * 128]), wba[:, kc, :],
                           start=(kc == 0), stop=(kc == 7))
                  P.cp('dve', baTM[:, i, :], ps[:, 0:8])
              if 'ba' in dbg and l == 0:
                  db = dbg_dram('ba', [128, NTILE, 8], F32)
                  P.dma('sp', db[:, :, :], baTM[:, :, :])
        if upto < 2:
            break
        S5O = dbg_dram('S5O%d' % l, [256, L], BF16)
        GD = dbg_dram('GD%d' % l, [512, L], BF16)
        MO = dbg_dram('MO%d' % l, [256, L], BF16)
        if 0 in branches:
            GQ = dbg_dram('GQ%d' % l, [12 * 128, L], BF16)
            gdn_prep_stage(l, PT, GQ)
            if not _os_env('GDN_PREP_ONLY'):
                gdn_stage(l, PT, GQ, GD, baTM)
        if 1 in branches:
            s5_stage(l, PT, S5O)
        if 2 in branches:
            moba_stage(l, PT, MO)
        if upto < 3:
            break
        Xn = dbg_dram('XM%d' % l, [D, L], F32)
        merge_stage(l, PT, GD, S5O, MO, Xcur, Xn, branches)
        Xcur = Xn
        if upto < 4:
            break
        if 3 in branches:
            if fake_xm and l == 0:
                Xcur = ein('XMin', [D, L])
            UTb = P.dram('UTb%d' % l, [D, 16384], BF16)
            Vb = P.dram('Vb%d' % l, [16384, D], BF16)
            H2 = P.dram('H2_%d' % l, [D, L], BF16)
            QS = dbg_dram('QS%d' % l, [2048, L], F32)
            Xo = dbg_dram('XO%d' % l, [D, L], F32)
            peer_cast_stage(l, UTb, Vb)
            peer_prep_stage(l, Xcur, H2, QS)
            peer_stage(l, Xcur, Xo, H2, QS, UTb, Vb, ntb=peer_ntb)
            Xcur = Xo

    with P.stage():
        fw = P.sbuf([128, 8], F32)
        P.dma('sp', fw[:, :], fnwT[:, :])
        xv = Xcur.re('(kc p) t -> p kc t', p=128)
        ov = outT.re('(kc p) t -> p kc t', p=128)
        xts = [P.sbuf([128, 8, 512], F32) for _ in range(2)]
        ots = [P.sbuf([128, 8, 512], F32) for _ in range(2)]
        sq = P.sbuf([128, 8, 512], F32)
        rs = P.sbuf([128, 512], F32)
        ssp = P.psum([128, 512])
        for tb in range(8):
            xt = xts[tb % 2]
            ot = ots[tb % 2]
            P.dma('sp', xt[:, :, :], xv[:, :, tb * 512:(tb + 1) * 512])
            P.act(sq[:, :, :], xt[:, :, :], AF.Square)
            for kc in range(8):
                P.mm(ssp[:, :], cm[:, C_ONES, :], sq[:, kc, :], start=(kc == 0), stop=(kc == 7))
            P.act(rs[:, :], ssp[:, :], AF.Sqrt, bias=1e-6, scale=1.0 / D)
            P.recip(rs[:, :], rs[:, :])
            for kc in range(8):
                P.stt(ot[:, kc, :], xt[:, kc, :], fw[:, kc:kc + 1], rs[:, :], ALU.mult, ALU.mult)
            P.dma('sp', ov[:, :, tb * 512:(tb + 1) * 512], ot[:, :, :])
    P.close()
    return nc, dbg_out


def prep_inputs(inp, b):
    f = lambda a: np.ascontiguousarray(a, dtype=np.float32)
    m = {}
    m['xT'] = f(inp['x'][b].T)
    m['cT'] = f(inp['c'][b].reshape(8, 128).T)
    m['cmats'] = _const_mats()
    cv = np.zeros((128, 4), np.float32)
    cv[:64, 0] = 1.0
    cv[64:, 1] = 1.0
    m['cvec'] = cv
    m['fnwT'] = f(inp['final_norm_w'].reshape(8, 128).T)
    pp = np.arange(128)[:, None, None]
    scc = np.arange(2)[None, :, None]
    tl = np.arange(256)[None, None, :]
    m['cmaskD'] = np.where(tl >= scc * 128 + pp, 0.0, -BIGRAW).astype(np.float32)
    st = np.zeros((128, 16, 128), np.float32)
    for n in range(16):
        st[n, n, :] = 1.0
    m['selTD'] = st
    for l in range(2):
        s = '_%d' % l
        m['ada_w' + s] = f(inp['ada_w'][l])
        m['ada_bT' + s] = f(inp['ada_b'][l].reshape(48, 128).T)
        m['n1T' + s] = f(inp['norm1_w'][l].reshape(8, 128).T)
        m['n2T' + s] = f(inp['norm2_w'][l].reshape(8, 128).T)
        wi = inp['w_in'][l]
        m['w_main' + s] = f(np.concatenate([wi[:, 0:2048], wi[:, 2056:6152]], axis=1))
        m['w_ba' + s] = f(wi[:, 2048:2056])
        q = np.arange(128)
        gi = 2 * np.arange(8)[None, :] + (q // 64)[:, None]
        pi = np.broadcast_to((q % 64)[:, None], (128, 8))
        m['s5_are' + s] = f(inp['s5_a_re'][l][gi, pi])
        m['s5_aim' + s] = f(inp['s5_a_im'][l][gi, pi])
        m['s5_ldt' + s] = f(inp['s5_log_dt'][l][gi])
        m['s5_bre' + s] = f(inp['s5_b_re'][l][gi, pi])
        m['s5_bim' + s] = f(inp['s5_b_im'][l][gi, pi])
        m['s5_ctre' + s] = f(inp['s5_c_re'][l].transpose(0, 2, 1))
        m['s5_ctim' + s] = f(inp['s5_c_im'][l].transpose(0, 2, 1))
        m['s5_dT' + s] = f(inp['s5_d'][l].reshape(2, 128).T)
        m['s5_gluw' + s] = f(inp['s5_glu_w'][l])
        m['s5_glubT' + s] = f(inp['s5_glu_b'][l].reshape(4, 128).T)
        m['wq' + s] = f(inp['peer_wq'][l])
        m['k1T' + s] = f(inp['peer_k1'][l].transpose(0, 2, 1))
        m['k2T' + s] = f(inp['peer_k2'][l].transpose(0, 2, 1))
        m['uT' + s] = f(inp['peer_u'][l].T)
        m['vtab' + s] = f(inp['peer_v'][l])
        m['convT' + s] = f(inp['gdn_conv_w'][l].reshape(4, 12, 128).transpose(2, 1, 0))
        m['gdn_ab' + s] = f(np.broadcast_to(np.concatenate([inp['gdn_a_log'][l], inp['gdn_dt_bias'][l]])[None, :], (128, 8)))
        m['gnw' + s] = f(inp['gdn_norm_w'][l].reshape(128, 1))
        m['wa' + s] = f(inp['w_branch_a'][l])
        m['wb' + s] = f(inp['w_branch_b'][l])
        m['wc' + s] = f(inp['w_branch_c'][l])
        m['wout' + s] = f(inp['w_out'][l])
    return m


def kernel(**inputs):
    nc, _ = build()
    in_maps = [prep_inputs(inputs, b) for b in range(8)]
    res = run_bass_kernel_spmd(nc, in_maps, core_ids=list(range(8)))
    out = np.stack([res.results[b]['outT'].T for b in range(8)], axis=0)
    return np.ascontiguousarray(out.astype(np.float32))
```

```python
import contextlib
import numpy as np
import concourse.bass as bass
import concourse.mybir as mybir
from concourse.bass_utils import run_bass_kernel_spmd

F32 = mybir.dt.float32
BF16 = mybir.dt.bfloat16
AF = mybir.ActivationFunctionType
ALU = mybir.AluOpType
AX = mybir.AxisListType

ENGS = ['pe', 'dve', 'act', 'pool', 'sp']
L = 4096
D = 1024
NTILE = 32
BIGRAW = 240000.0


class View:
    __slots__ = ('b', 'ap')

    def __init__(self, b, ap):
        self.b = b
        self.ap = ap

    def bc(self, shape):
        return View(self.b, self.ap.broadcast_to(list(shape)))

    def re(self, pat, **kw):
        return View(self.b, self.ap.rearrange(pat, **kw))

    def __getitem__(self, idx):
        return View(self.b, self.ap[idx])


class Buf:
    __slots__ = ('t', 'w', 'r', 'name', 'ps')

    def __init__(self, t=None, name=''):
        self.t = t
        self.ps = False
        self.w = {}
        self.r = {}
        self.name = name

    def __getitem__(self, idx):
        return View(self, self.t[idx])

    def re(self, pat, **kw):
        return View(self, self.t.rearrange(pat, **kw))

    def alias(self):
        b = Buf(self.t, self.name + '_a')
        b.ps = self.ps
        return b


class Prog:
    def __init__(self, nc, n_dma_sems=24):
        self.nc = nc
        self.es = contextlib.ExitStack()
        self.ses = None
        self.ops = {e: [] for e in ENGS}
        self.cnt = {e: 0 for e in ENGS}
        self.sem = {}
        for e in ['pe', 'dve', 'act', 'pool']:
            self.sem[e] = self.es.enter_context(nc.semaphore('s_' + e))
        self.dsem = [self.es.enter_context(nc.semaphore('d%d' % i)) for i in range(n_dma_sems)]
        self.dcum = [0] * n_dma_sems
        self.dnext = 0
        self.known = {e: {} for e in ENGS}
        self._uid = 0
        self.ninstr = 0

    def sbuf(self, shape, dt, name=None, persist=False):
        self._uid += 1
        name = name or ('sb%d' % self._uid)
        es = self.es if (persist or self.ses is None) else self.ses
        t = es.enter_context(self.nc.sbuf_tensor(name, list(shape), dt))
        return Buf(t, name)

    def psum(self, shape, dt=F32, name=None):
        self._uid += 1
        name = name or ('ps%d' % self._uid)
        es = self.es if self.ses is None else self.ses
        t = es.enter_context(self.nc.psum_tensor(name, list(shape), dt))
        b = Buf(t, name)
        b.ps = True
        return b

    def dram(self, name, shape, dt, kind='Internal'):
        t = self.nc.dram_tensor(name, list(shape), dt, kind=kind)
        return Buf(t.ap(), name)

    def _semobj(self, key):
        return self.sem[key] if isinstance(key, str) else self.dsem[key]

    def _collect(self, eng, reads, writes):
        need = {}
        for b in reads:
            for k, v in b.w.items():
                if need.get(k, 0) < v:
                    need[k] = v
            if b.ps:
                for k, v in b.r.items():
                    if k != eng and need.get(k, 0) < v:
                        need[k] = v
        for b in writes:
            for k, v in b.w.items():
                if need.get(k, 0) < v:
                    need[k] = v
            for k, v in b.r.items():
                if need.get(k, 0) < v:
                    need[k] = v
        waits = []
        kn = self.known[eng]
        for k, v in need.items():
            if k == 'pe' and eng == 'pe':
                continue
            if kn.get(k, 0) >= v:
                continue
            kn[k] = v
            waits.append((k, v))
        return waits

    def op(self, eng, fn, reads=(), writes=()):
        waits = self._collect(eng, reads, writes)
        self.cnt[eng] += 1
        idx = self.cnt[eng]
        self.ops[eng].append((waits, fn, (eng, 1)))
        for b in writes:
            b.w = {eng: idx}
            b.r = {}
        for b in reads:
            if b.r.get(eng, 0) < idx:
                b.r[eng] = idx
        self.ninstr += 1

    def dma(self, eng, out, in_, **kw):
        reads, writes = [in_.b], [out.b]
        waits = self._collect(eng, reads, writes)
        k = self.dnext
        self.dnext = (self.dnext + 1) % len(self.dsem)
        if self.dcum[k] > 0 and self.known[eng].get(k, 0) < self.dcum[k]:
            self.known[eng][k] = self.dcum[k]
            waits.append((k, self.dcum[k]))
        self.dcum[k] += 16
        val = self.dcum[k]
        oa, ia = out.ap, in_.ap

        def fn(e):
            return e.dma_start(out=oa, in_=ia, **kw)
        self.ops[eng].append((waits, fn, (k, 16)))
        for b in writes:
            b.w = {k: val}
            b.r = {}
        for b in reads:
            if b.r.get(k, 0) < val:
                b.r[k] = val
        self.ninstr += 1

    @contextlib.contextmanager
    def stage(self):
        self.ses = contextlib.ExitStack()
        try:
            yield
            self.flush()
        finally:
            self.ses.close()
            self.ses = None

    def flush(self):
        waits = []
        for k in range(len(self.dsem)):
            if self.dcum[k] > 0 and self.known['sp'].get(k, 0) < self.dcum[k]:
                self.known['sp'][k] = self.dcum[k]
                waits.append((k, self.dcum[k]))
        self.ops['sp'].append((waits, None, None))
        nc = self.nc
        with nc.Block() as block:
            def mk(ename):
                def body(e):
                    for waits, fn, inc in self.ops[ename]:
                        for k, v in waits:
                            e.wait_ge(self._semobj(k), v)
                        if fn is None:
                            continue
                        ins = fn(e)
                        ins.then_inc(self._semobj(inc[0]), inc[1])
                return body
            block.tensor(mk('pe'))
            block.vector(mk('dve'))
            block.scalar(mk('act'))
            block.gpsimd(mk('pool'))
            block.sync(mk('sp'))
        self.ops = {e: [] for e in ENGS}
        for e in ENGS:
            for e2 in ['pe', 'dve', 'act', 'pool']:
                self.known[e][e2] = self.cnt[e2]
            for k in range(len(self.dsem)):
                self.known[e][k] = self.dcum[k]

    def close(self):
        self.es.close()

    def mm(self, out, lhsT, rhs, start=True, stop=True):
        self.op('pe', lambda e: e.matmul(out.ap, lhsT=lhsT.ap, rhs=rhs.ap, start=start, stop=stop),
                reads=[lhsT.b, rhs.b], writes=[out.b])

    def tr(self, out, in_, ident):
        self.op('pe', lambda e: e.transpose(out.ap, in_.ap, ident.ap), reads=[in_.b, ident.b], writes=[out.b])

    def act(self, out, in_, func, bias=None, scale=None, accum=None):
        reads = [in_.b]
        kw = {}
        if bias is not None:
            if isinstance(bias, View):
                reads.append(bias.b)
                kw['bias'] = bias.ap
            else:
                kw['bias'] = float(bias)
        if scale is not None:
            if isinstance(scale, View):
                reads.append(scale.b)
                kw['scale'] = scale.ap
            else:
                kw['scale'] = float(scale)
        if func == AF.Copy and isinstance(scale, View):
            func = AF.Identity
        writes = [out.b]
        if accum is not None:
            kw['accum_out'] = accum.ap
            writes.append(accum.b)
        self.op('act', lambda e: e.activation(out=out.ap, in_=in_.ap, func=func, **kw), reads=reads, writes=writes)

    def tt(self, eng, out, a, b, op):
        self.op(eng, lambda e: e.tensor_tensor(out=out.ap, in0=a.ap, in1=b.ap, op=op), reads=[a.b, b.b], writes=[out.b])

    def ts(self, eng, out, a, s1, op0, s2=None, op1=None):
        reads = [a.b]
        s1v = s1.ap if isinstance(s1, View) else float(s1)
        if isinstance(s1, View):
            reads.append(s1.b)
        s2v = None
        if s2 is not None:
            s2v = s2.ap if isinstance(s2, View) else float(s2)
            if isinstance(s2, View):
                reads.append(s2.b)
        if op1 is None:
            self.op(eng, lambda e: e.tensor_scalar(out=out.ap, in0=a.ap, scalar1=s1v, scalar2=None, op0=op0),
                    reads=reads, writes=[out.b])
        else:
            self.op(eng, lambda e: e.tensor_scalar(out=out.ap, in0=a.ap, scalar1=s1v, scalar2=s2v, op0=op0, op1=op1),
                    reads=reads, writes=[out.b])

    def stt(self, out, in0, scalar, in1, op0, op1):
        reads = [in0.b, in1.b]
        sv = scalar.ap if isinstance(scalar, View) else float(scalar)
        if isinstance(scalar, View):
            reads.append(scalar.b)
        self.op('dve', lambda e: e.scalar_tensor_tensor(out=out.ap, in0=in0.ap, scalar=sv, in1=in1.ap, op0=op0, op1=op1),
                reads=reads, writes=[out.b])

    def cp(self, eng, out, in_):
        if eng == 'act':
            self.op('act', lambda e: e.activation(out=out.ap, in_=in_.ap, func=AF.Copy), reads=[in_.b], writes=[out.b])
        else:
            self.op(eng, lambda e: e.tensor_copy(out=out.ap, in_=in_.ap), reads=[in_.b], writes=[out.b])

    def memset(self, eng, out, val):
        self.op(eng, lambda e: e.memset(out.ap, val), writes=[out.b])

    def recip(self, out, in_):
        self.op('dve', lambda e: e.reciprocal(out=out.ap, in_=in_.ap), reads=[in_.b], writes=[out.b])


def _const_mats():
    idx = np.arange(128)
    same = (idx[:, None] // 64) == (idx[None, :] // 64)
    ident = np.eye(128, dtype=np.float32)
    L2 = (same & (idx[:, None] >= idx[None, :])).astype(np.float32)
    SL2 = (same & (idx[:, None] > idx[None, :])).astype(np.float32)
    U2 = L2.T.copy()
    B2 = same.astype(np.float32)
    ones = np.ones((128, 128), np.float32)
    cm = np.stack([ident, L2, SL2, U2, B2, ones], axis=1)
    return np.ascontiguousarray(cm)


C_ID, C_L2, C_SL2, C_U2, C_B2, C_ONES = range(6)


def _os_env(k):
    import os
    return os.environ.get(k)


def build(upto=99, dbg=(), branches=(0, 1, 2, 3), fake_pt=False, fake_xm=False, peer_ntb=16, peer_only=False):
    nc = bass.Bass('TRN2', target_bir_lowering=False)
    P = Prog(nc)
    ein = lambda n, s, dt=F32: P.dram(n, s, dt, kind='ExternalInput')
    xT = ein('xT', [D, L])
    cT = ein('cT', [128, 8])
    cmats = ein('cmats', [128, 6, 128])
    cvec = ein('cvec', [128, 4])
    fnwT = ein('fnwT', [128, 8])
    cmaskD = ein('cmaskD', [128, 2, 256])
    selTD = ein('selTD', [128, 16, 128])
    W = []
    for l in range(2):
        w = {}
        s = '_%d' % l
        w['ada_w'] = ein('ada_w' + s, [D, 6 * D])
        w['ada_bT'] = ein('ada_bT' + s, [128, 48])
        w['n1T'] = ein('n1T' + s, [128, 8])
        w['n2T'] = ein('n2T' + s, [128, 8])
        w['w_main'] = ein('w_main' + s, [D, 6144])
        w['w_ba'] = ein('w_ba' + s, [D, 8])
        for nm in ['s5_are', 's5_aim', 's5_ldt']:
            w[nm] = ein(nm + s, [128, 8])
        w['s5_bre'] = ein('s5_bre' + s, [128, 8, 16])
        w['s5_bim'] = ein('s5_bim' + s, [128, 8, 16])
        w['s5_ctre'] = ein('s5_ctre' + s, [16, 64, 16])
        w['s5_ctim'] = ein('s5_ctim' + s, [16, 64, 16])
        w['s5_dT'] = ein('s5_dT' + s, [128, 2])
        w['s5_gluw'] = ein('s5_gluw' + s, [256, 512])
        w['s5_glubT'] = ein('s5_glubT' + s, [128, 4])
        w['wq'] = ein('wq' + s, [D, 2048])
        w['k1T'] = ein('k1T' + s, [8, 128, 128])
        w['k2T'] = ein('k2T' + s, [8, 128, 128])
        w['uT'] = ein('uT' + s, [D, 16384])
        w['vtab'] = ein('vtab' + s, [16384, D])
        w['convT'] = ein('convT' + s, [128, 12, 4])
        w['gdn_ab'] = ein('gdn_ab' + s, [128, 8])
        w['gnw'] = ein('gnw' + s, [128, 1])
        w['wa'] = ein('wa' + s, [512, D])
        w['wb'] = ein('wb' + s, [256, D])
        w['wc'] = ein('wc' + s, [256, D])
        w['wout'] = ein('wout' + s, [D, D])
        W.append(w)
    outT = P.dram('outT', [D, L], F32, kind='ExternalOutput')
    dbg_out = {}

    def dbg_dram(name, shape, dt):
        if name in dbg:
            b = P.dram('dbg_' + name, shape, dt, kind='ExternalOutput')
            dbg_out[name] = b
            return b
        return P.dram('scr_' + name, shape, dt)

    mod = [P.sbuf([128, 48], F32, persist=True) for _ in range(2)]
    A1 = [P.sbuf([128, 8], F32, persist=True) for _ in range(2)]
    A2 = [P.sbuf([128, 8], F32, persist=True) for _ in range(2)]
    cm = P.sbuf([128, 6, 128], F32, persist=True)
    cv = P.sbuf([128, 4], F32, persist=True)
    identb = P.sbuf([128, 128], BF16, persist=True)
    onesb = P.sbuf([128, 128], BF16, persist=True)

    with P.stage():
        P.dma('sp', cm[:, :, :], cmats[:, :, :])
        P.dma('sp', cv[:, :], cvec[:, :])
        P.cp('dve', identb[:, :], cm[:, C_ID, :])
        P.cp('dve', onesb[:, :], cm[:, C_ONES, :])
        ct = P.sbuf([128, 8], F32)
        sc_ = P.sbuf([128, 8], F32)
        P.dma('sp', ct[:, :], cT[:, :])
        P.act(sc_[:, :], ct[:, :], AF.Silu)
        wbuf = [P.sbuf([128, 8, 1024], F32) for _ in range(2)]
        mps = P.psum([128, 512])
        for l in range(2):
            bT = P.sbuf([128, 48], F32)
            n1 = P.sbuf([128, 8], F32)
            n2 = P.sbuf([128, 8], F32)
            P.dma('sp', bT[:, :], W[l]['ada_bT'][:, :])
            P.dma('sp', n1[:, :], W[l]['n1T'][:, :])
            P.dma('sp', n2[:, :], W[l]['n2T'][:, :])
            awv = W[l]['ada_w'].re('(kc p) j -> p kc j', p=128)
            for jg in range(6):
                wb = wbuf[jg % 2]
                P.dma('sp', wb[:, :, :], awv[:, :, jg * 1024:(jg + 1) * 1024])
                for jc in range(8):
                    col = jg * 8 + jc
                    for kc in range(8):
                        P.mm(mps[:, col:col + 1], wb[:, kc, jc * 128:(jc + 1) * 128], sc_[:, kc:kc + 1],
                             start=(kc == 0), stop=(kc == 7))
            P.tt('dve', mod[l][:, :], mps[:, 0:48], bT[:, :], ALU.add)
            P.stt(A1[l][:, :], mod[l][:, 8:16], 1.0, n1[:, :], ALU.add, ALU.mult)
            P.stt(A2[l][:, :], mod[l][:, 32:40], 1.0, n2[:, :], ALU.add, ALU.mult)
        if 'mod' in dbg:
            dm = dbg_dram('mod', [128, 96], F32)
            P.dma('sp', dm[:, 0:48], mod[0][:, :])
            P.dma('sp', dm[:, 48:96], mod[1][:, :])

    def norm_mod(Xsrc, A, B, hT_blks, hT):
        xv = Xsrc.re('(kc p) t -> p kc t', p=128)
        xts = [P.sbuf([128, 8, 512], F32) for _ in range(2)]
        sq = P.sbuf([128, 8, 512], F32)
        rs = P.sbuf([128, 512], F32)
        tmp = [P.sbuf([128, 512], F32) for _ in range(2)]
        ssp = P.psum([128, 512])
        for tb in range(8):
            xt = xts[tb % 2]
            P.dma('sp', xt[:, :, :], xv[:, :, tb * 512:(tb + 1) * 512])
            P.act(sq[:, :, :], xt[:, :, :], AF.Square)
            for kc in range(8):
                P.mm(ssp[:, :], cm[:, C_ONES, :], sq[:, kc, :], start=(kc == 0), stop=(kc == 7))
            P.act(rs[:, :], ssp[:, :], AF.Sqrt, bias=1e-6, scale=1.0 / D)
            P.recip(rs[:, :], rs[:, :])
            hb = hT_blks[tb]
            for kc in range(8):
                t_ = tmp[kc % 2]
                P.stt(t_[:, :], xt[:, kc, :], A[:, kc:kc + 1], rs[:, :], ALU.mult, ALU.mult)
                P.act(View(hb, hT.t[:, kc, tb * 512:(tb + 1) * 512]), t_[:, :], AF.Identity, bias=B[:, kc:kc + 1])

    TWO_PI = 2.0 * np.pi

    def s5_stage(l, PT, S5O):
        w = W[l]
        with P.stage():
            sm = lambda: P.sbuf([128, 8], F32)
            are, aim, ldt = sm(), sm(), sm()
            P.dma('sp', are[:, :], w['s5_are'][:, :])
            P.dma('sp', aim[:, :], w['s5_aim'][:, :])
            P.dma('sp', ldt[:, :], w['s5_ldt'][:, :])
            dt, lr, th, r = sm(), sm(), sm(), sm()
            P.act(dt[:, :], ldt[:, :], AF.Exp)
            P.tt('dve', lr[:, :], are[:, :], dt[:, :], ALU.mult)
            P.tt('dve', th[:, :], aim[:, :], dt[:, :], ALU.mult)
            P.act(r[:, :], lr[:, :], AF.Exp)
            ki = P.sbuf([128, 8], mybir.dt.int32)

            def sin_of(src, dst):
                t0, t1, t2 = sm(), sm(), sm()
                P.ts('dve', t0[:, :], src[:, :], 1.0 / TWO_PI, ALU.mult)
                P.cp('dve', ki[:, :], t0[:, :])
                P.cp('dve', t1[:, :], ki[:, :])
                P.stt(t2[:, :], t1[:, :], -TWO_PI, src[:, :], ALU.mult, ALU.add)
                P.ts('dve', t0[:, :], t2[:, :], float(np.pi), ALU.is_gt)
                P.stt(t1[:, :], t0[:, :], -TWO_PI, t2[:, :], ALU.mult, ALU.add)
                P.ts('dve', t0[:, :], t1[:, :], float(-np.pi), ALU.is_lt)
                P.stt(t2[:, :], t0[:, :], TWO_PI, t1[:, :], ALU.mult, ALU.add)
                P.act(dst[:, :], t2[:, :], AF.Sin)
            sn, cs, thc = sm(), sm(), sm()
            sin_of(th, sn)
            P.ts('dve', thc[:, :], th[:, :], float(np.pi / 2), ALU.add)
            sin_of(thc, cs)
            ar, ai, arm1, den, cre, cim, ncim, t0, t1 = sm(), sm(), sm(), sm(), sm(), sm(), sm(), sm(), sm()
            P.tt('dve', ar[:, :], r[:, :], cs[:, :], ALU.mult)
            P.tt('dve', ai[:, :], r[:, :], sn[:, :], ALU.mult)
            P.ts('dve', arm1[:, :], ar[:, :], -1.0, ALU.add)
            P.tt('dve', t0[:, :], are[:, :], are[:, :], ALU.mult)
            P.tt('dve', t1[:, :], aim[:, :], aim[:, :], ALU.mult)
            P.tt('dve', den[:, :], t0[:, :], t1[:, :], ALU.add)
            P.recip(den[:, :], den[:, :])
            P.tt('dve', t0[:, :], arm1[:, :], are[:, :], ALU.mult)
            P.tt('dve', t1[:, :], ai[:, :], aim[:, :], ALU.mult)
            P.tt('dve', t0[:, :], t0[:, :], t1[:, :], ALU.add)
            P.tt('dve', cre[:, :], t0[:, :], den[:, :], ALU.mult)
            P.tt('dve', t0[:, :], ai[:, :], are[:, :], ALU.mult)
            P.tt('dve', t1[:, :], arm1[:, :], aim[:, :], ALU.mult)
            P.tt('dve', t0[:, :], t0[:, :], t1[:, :], ALU.subtract)
            P.tt('dve', cim[:, :], t0[:, :], den[:, :], ALU.mult)
            P.ts('dve', ncim[:, :], cim[:, :], -1.0, ALU.mult)
            Bre = P.sbuf([128, 8, 16], F32)
            Bim = P.sbuf([128, 8, 16], F32)
            P.dma('sp', Bre[:, :, :], w['s5_bre'][:, :, :])
            P.dma('sp', Bim[:, :, :], w['s5_bim'][:, :, :])
            bbre = P.sbuf([128, 8, 16], F32)
            bbim = P.sbuf([128, 8, 16], F32)
            tb16 = P.sbuf([128, 16], F32)
            for s_ in range(8):
                P.ts('dve', tb16[:, :], Bre[:, s_, :], cre[:, s_:s_ + 1], ALU.mult)
                P.stt(bbre[:, s_, :], Bim[:, s_, :], ncim[:, s_:s_ + 1], tb16[:, :], ALU.mult, ALU.add)
                P.ts('dve', tb16[:, :], Bim[:, s_, :], cre[:, s_:s_ + 1], ALU.mult)
                P.stt(bbim[:, s_, :], Bre[:, s_, :], cim[:, s_:s_ + 1], tb16[:, :], ALU.mult, ALU.add)
            BbT = [[P.sbuf([128, 128], F32) for _ in range(8)] for _ in range(2)]
            CT = [[P.sbuf([128, 128], F32) for _ in range(8)] for _ in range(2)]
            stg = [P.sbuf([16, 128], F32) for _ in range(2)]
            psA = P.psum([128, 512])
            psB = P.psum([128, 512])
            for ri, bb in enumerate([bbre, bbim]):
                for s_ in range(8):
                    P.memset('pool', BbT[ri][s_][:, :], 0.0)
                    P.memset('pool', CT[ri][s_][:, :], 0.0)
                    st = stg[s_ % 2]
                    P.tr(psA[0:16, 0:128], bb[:, s_, :], cm[:, C_ID, :])
                    P.cp('act', st[:, :], psA[0:16, 0:128])
                    for g2 in range(2):
                        g = 2 * s_ + g2
                        r0 = (g % 8) * 16
                        P.dma('sp', BbT[ri][s_][r0:r0 + 16, g2 * 64:(g2 + 1) * 64], st[0:16, g2 * 64:(g2 + 1) * 64])
                        src = w['s5_ctre' if ri == 0 else 's5_ctim']
                        P.dma('sp', CT[ri][s_][g2 * 64:(g2 + 1) * 64, r0:r0 + 16], src[g, :, :])
            for s_ in range(8):
                P.ts('dve', CT[1][s_][:, :], CT[1][s_][:, :], -1.0, ALU.mult)
            Cj = [P.sbuf([128, 512], F32) for _ in range(8)]
            Sj = [P.sbuf([128, 512], F32) for _ in range(8)]
            rfull = [P.sbuf([128, 512], F32) for _ in range(8)]
            nsn = sm()
            tmpA = P.sbuf([128, 256], F32)
            tmpB = P.sbuf([128, 256], F32)
            for s_ in range(8):
                P.cp('dve', Cj[s_][:, 0:1], cs[:, s_:s_ + 1])
                P.cp('dve', Sj[s_][:, 0:1], sn[:, s_:s_ + 1])
                P.memset('pool', rfull[s_][:, :], 0.0)
                P.ts('pool', rfull[s_][:, :], rfull[s_][:, :], r[:, s_:s_ + 1], ALU.add)
                n = 1
                while n < 512:
                    c_n = Cj[s_][:, n - 1:n]
                    s_n = Sj[s_][:, n - 1:n]
                    P.ts('dve', nsn[:, 0:1], s_n, -1.0, ALU.mult)
                    P.ts('dve', tmpA[:, 0:n], Cj[s_][:, 0:n], c_n, ALU.mult)
                    P.ts('dve', tmpB[:, 0:n], Cj[s_][:, 0:n], s_n, ALU.mult)
                    P.stt(Cj[s_][:, n:2 * n], Sj[s_][:, 0:n], nsn[:, 0:1], tmpA[:, 0:n], ALU.mult, ALU.add)
                    P.stt(Sj[s_][:, n:2 * n], Sj[s_][:, 0:n], c_n, tmpB[:, 0:n], ALU.mult, ALU.add)
                    n *= 2
            ub = P.sbuf([128, 2, L], BF16)
            uF = P.sbuf([128, 2, L], F32)
            P.dma('sp', ub[:, :, :], PT.re('(c p) t -> p c t', p=128)[:, 16:18, :])
            P.cp('pool', uF[:, 0, :], ub[:, 0, :])
            P.cp('act', uF[:, 1, :], ub[:, 1, :])
            dcol = P.sbuf([128, 2], F32)
            P.dma('sp', dcol[:, :], w['s5_dT'][:, :])
            gwf = P.sbuf([128, 2, 512], F32)
            gw = P.sbuf([128, 2, 512], BF16)
            P.dma('sp', gwf[:, :, :], w['s5_gluw'].re('(kc p) j -> p kc j', p=128))
            P.cp('dve', gw[:, :, :], gwf[:, :, :])
            gb = P.sbuf([128, 4], F32)
            P.dma('sp', gb[:, :], w['s5_glubT'][:, :])
            Sre = [P.sbuf([128, 512], F32) for _ in range(8)]
            Sim = [P.sbuf([128, 512], F32) for _ in range(8)]
            wk = [P.sbuf([128, 512], F32) for _ in range(8)]
            psC = P.psum([128, 512])
            psD = P.psum([128, 512])
            psY = P.psum([128, 512])
            psG = [P.psum([128, 512]) for _ in range(2)]
            yv = P.sbuf([128, 512], F32)
            zg = P.sbuf([128, 2, 512], BF16)
            sig = P.sbuf([128, 512], F32)
            so = [P.sbuf([128, 2, 512], BF16) for _ in range(2)]
            s5v = S5O.re('(c p) t -> p c t', p=128)
            for tb in range(8):
                tsl = slice(tb * 512, (tb + 1) * 512)
                for s_ in range(8):
                    ch = s_ // 4
                    p1, p2 = (psA, psB) if s_ % 2 == 0 else (psC, psD)
                    P.mm(p1[:, :], BbT[0][s_][:, :], uF[:, ch, tsl])
                    P.mm(p2[:, :], BbT[1][s_][:, :], uF[:, ch, tsl])
                    t1, t2, t3, t4, zre, zim, hre, him = wk
                    P.tt('dve', t1[:, :], p1[:, :], Cj[s_][:, :], ALU.mult)
                    P.tt('dve', t2[:, :], p2[:, :], Sj[s_][:, :], ALU.mult)
                    P.tt('pool', zre[:, :], t1[:, :], t2[:, :], ALU.add)
                    P.tt('dve', t3[:, :], p2[:, :], Cj[s_][:, :], ALU.mult)
                    P.tt('dve', t4[:, :], p1[:, :], Sj[s_][:, :], ALU.mult)
                    P.tt('pool', zim[:, :], t3[:, :], t4[:, :], ALU.subtract)
                    for (z, h, Sx) in ((zre, hre, Sre[s_]), (zim, him, Sim[s_])):
                        if tb == 0:
                            P.op('dve', lambda e, h=h, z=z, rf=rfull[s_]: e.tensor_tensor_scan(
                                out=h.t[:, :], data0=rf.t[:, :], data1=z.t[:, :], initial=0.0, op0=ALU.mult, op1=ALU.add),
                                reads=[rfull[s_], z], writes=[h])
                        else:
                            P.op('dve', lambda e, h=h, z=z, rf=rfull[s_], Sx=Sx: e.tensor_tensor_scan(
                                out=h.t[:, :], data0=rf.t[:, :], data1=z.t[:, :], initial=Sx.t[:, 511:512],
                                op0=ALU.mult, op1=ALU.add), reads=[rfull[s_], z, Sx], writes=[h])
                    P.tt('dve', t1[:, :], hre[:, :], Cj[s_][:, :], ALU.mult)
                    P.tt('dve', t2[:, :], him[:, :], Sj[s_][:, :], ALU.mult)
                    P.tt('pool', Sre[s_][:, :], t1[:, :], t2[:, :], ALU.subtract)
                    P.tt('dve', t3[:, :], hre[:, :], Sj[s_][:, :], ALU.mult)
                    P.tt('dve', t4[:, :], him[:, :], Cj[s_][:, :], ALU.mult)
                    P.tt('pool', Sim[s_][:, :], t3[:, :], t4[:, :], ALU.add)
                for ch in range(2):
                    k = 0
                    for s_ in range(ch * 4, ch * 4 + 4):
                        P.mm(psY[:, :], CT[0][s_][:, :], Sre[s_][:, :], start=(k == 0), stop=False)
                        P.mm(psY[:, :], CT[1][s_][:, :], Sim[s_][:, :], start=False, stop=(k == 3))
                        k += 1
                    P.stt(yv[:, :], uF[:, ch, tsl], dcol[:, ch:ch + 1], psY[:, :], ALU.mult, ALU.add)
                    P.act(zg[:, ch, :], yv[:, :], AF.Gelu)
                sob = so[tb % 2]
                for oc in range(2):
                    for kc in range(2):
                        P.mm(psG[0][:, :], gw[:, kc, oc * 128:(oc + 1) * 128], zg[:, kc, :], start=(kc == 0), stop=(kc == 1))
                    for kc in range(2):
                        P.mm(psG[1][:, :], gw[:, kc, (oc + 2) * 128:(oc + 3) * 128], zg[:, kc, :], start=(kc == 0), stop=(kc == 1))
                    P.act(sig[:, :], psG[1][:, :], AF.Sigmoid, bias=gb[:, oc + 2:oc + 3])
                    P.stt(sob[:, oc, :], psG[0][:, :], gb[:, oc:oc + 1], sig[:, :], ALU.add, ALU.mult)
                P.dma('sp', s5v[:, :, tsl], sob[:, :, :])

    def gdn_prep_stage(l, PT, GQ):
        w = W[l]
        with P.stage():
            ptv = PT.re('(c p) t -> p c t', p=128)
            gqv = GQ.re('(c p) t -> p c t', p=128)
            convw = P.sbuf([128, 12, 4], F32)
            P.dma('sp', convw[:, :, :], w['convT'][:, :, :])
            xin = [P.sbuf([128, L], BF16) for _ in range(2)]
            outb = [P.sbuf([128, L], BF16) for _ in range(2)]
            acc = P.sbuf([128, L], F32)
            qs = P.sbuf([128, L], F32)
            sq = P.sbuf([128, L], BF16)
            rn = [P.sbuf([128, 512], F32) for _ in range(2)]
            pss = [P.psum([128, 512]) for _ in range(2)]
            for cc in range(12):
                x = xin[cc % 2]
                ob = outb[cc % 2]
                P.dma('sp', x[:, :], ptv[:, cc, :])
                P.ts('dve', acc[:, :], x[:, :], convw[:, cc, 3:4], ALU.mult)
                for j in (2, 1, 0):
                    sh = 3 - j
                    P.stt(acc[:, sh:L], x[:, 0:L - sh], convw[:, cc, j:j + 1], acc[:, sh:L], ALU.mult, ALU.add)
                if cc >= 8:
                    P.act(ob[:, :], acc[:, :], AF.Silu)
                else:
                    P.act(qs[:, :], acc[:, :], AF.Silu)
                    P.act(sq[:, :], qs[:, :], AF.Square)
                    for tb in range(8):
                        tsl = slice(tb * 512, (tb + 1) * 512)
                        ps = pss[tb % 2]
                        r_ = rn[tb % 2]
                        P.mm(ps[:, :], onesb[:, :], sq[:, tsl])
                        P.act(r_[:, :], ps[:, :], AF.Sqrt, bias=1e-6)
                        P.recip(r_[:, :], r_[:, :])
                        if cc < 4:
                            P.stt(ob[:, tsl], qs[:, tsl], float(128 ** -0.5), r_[:, :], ALU.mult, ALU.mult)
                        else:
                            P.tt('dve', ob[:, tsl], qs[:, tsl], r_[:, :], ALU.mult)
                P.dma('sp', gqv[:, cc, :], ob[:, :])

    def gdn_stage(l, PT, GQ, GD, baTM):
        w = W[l]
        with P.stage():
            gqv = GQ.re('(c p) t -> p c t', p=128)
            ptv = PT.re('(c p) t -> p c t', p=128)
            gdv = GD.re('(c p) t -> p c t', p=128)
            qkv = P.sbuf([128, 12, L], BF16)
            qkvh = [qkv.alias() for _ in range(12)]
            for cc in range(12):
                P.dma('sp', View(qkvh[cc], qkv.t[:, cc, :]), gqv[:, cc, :])
            gab = P.sbuf([128, 8], F32)
            gnw = P.sbuf([128, 1], F32)
            P.dma('sp', gab[:, :], w['gdn_ab'][:, :])
            P.dma('sp', gnw[:, :], w['gnw'][:, :])
            T3 = lambda: P.sbuf([128, NTILE, 4], F32)
            M = lambda: P.sbuf([128, 128], F32)
            bet, xg, g3 = T3(), T3(), T3()
            P.act(bet[:, :, :], baTM[:, :, 0:4], AF.Sigmoid)
            P.tt('dve', xg[:, :, :], baTM[:, :, 4:8], gab[:, 4:8].re('p (o h) -> p o h', o=1).bc([128, NTILE, 4]), ALU.add)
            P.act(xg[:, :, :], xg[:, :, :], AF.Exp)
            P.act(xg[:, :, :], xg[:, :, :], AF.Ln, bias=1.0)
            ea = P.sbuf([128, 4], F32)
            P.act(ea[:, :], gab[:, 0:4], AF.Exp)
            P.tt('dve', g3[:, :, :], xg[:, :, :], ea[:, :].re('p (o h) -> p o h', o=1).bc([128, NTILE, 4]), ALU.mult)
            P.ts('dve', g3[:, :, :], g3[:, :, :], -1.0, ALU.mult)
            f2 = lambda b: View(b, b.t[:, :, :].rearrange('p i h -> p (i h)'))
            gcol = f2(g3)
            betc = f2(bet)
            pbank = [P.psum([128, 512]) for _ in range(7)]
            psbb = P.psum([128, 1024], BF16)
            gc, gl, egc, edl, nbeta, bge, ng = M(), M(), M(), M(), M(), M(), M()
            P.mm(pbank[0][:, 0:128], cm[:, C_U2, :], gcol)
            P.cp('dve', gc[:, :], pbank[0][:, 0:128])
            P.mm(pbank[0][:, 128:256], cm[:, C_B2, :], gcol)
            P.cp('dve', gl[:, :], pbank[0][:, 128:256])
            P.act(egc[:, :], gc[:, :], AF.Exp)
            P.tt('dve', edl[:, :], gl[:, :], gc[:, :], ALU.subtract)
            P.act(edl[:, :], edl[:, :], AF.Exp)
            P.ts('dve', nbeta[:, :], betc, -1.0, ALU.mult)
            P.tt('dve', bge[:, :], betc, egc[:, :], ALU.mult)
            P.ts('dve', ng[:, :], gcol, -1.0, ALU.mult)
            edl0, edl1 = M(), M()
            P.ts('dve', edl0[:, :], edl[:, :], cv[:, 0:1], ALU.mult)
            P.ts('dve', edl1[:, :], edl[:, :], cv[:, 1:2], ALU.mult)
            gm = P.sbuf([128, 2, 128], F32)
            P.ts('dve', gm[:, 0, :], gcol, cv[:, 0:1], ALU.mult)
            P.ts('dve', gm[:, 1, :], gcol, cv[:, 1:2], ALU.mult)
            EGL = P.sbuf([128, 2, 128], F32)
            P.mm(pbank[1][:, 0:256], cm[:, C_ONES, :], View(gm, gm.t[:, :, :].rearrange('p a b -> p (a b)')))
            P.act(View(EGL, EGL.t[:, :, :].rearrange('p a b -> p (a b)')), pbank[1][:, 0:256], AF.Exp)
            H = []
            for h in range(4):
                d = {}
                for nm in ['GU', 'nG', 'Dm', 'Dn', 'dec', 'decT', 'N', 'pA', 'pB', 'pTA', 'pTB', 'tT', 'usb', 'S', 'osb', 'osq']:
                    d[nm] = P.sbuf([128, 128], F32)
                for nm in ['bgk', 'kd0', 'kd1', 'bv', 'TTb', 'wT', 'qkm', 'Sbf', 'vnew', 'on', 'zt', 'zs', 'og']:
                    d[nm] = P.sbuf([128, 128], BF16)
                d['ss'] = P.sbuf([128, 1], F32)
                d['rr'] = P.sbuf([128, 1], F32)
                d['ps'] = []
                for k in range(7):
                    idx = h * 7 + k
                    bk = pbank[idx // 4]
                    d['ps'].append(View(bk, bk.t[:, (idx % 4) * 128:(idx % 4 + 1) * 128]))
                d['pb'] = [View(psbb, psbb.t[:, (2 * h + k) * 128:(2 * h + k + 1) * 128]) for k in range(2)]
                P.memset('pool', d['S'][:, :], 0.0)
                P.memset('pool', d['Sbf'][:, :], 0.0)
                P.memset('pool', d['vnew'][:, :], 0.0)
                H.append(d)
            U2 = cm[:, C_U2, :]
            B2 = cm[:, C_B2, :]
            SL2 = cm[:, C_SL2, :]
            IDf = cm[:, C_ID, :]
            _lim = int(_os_env('GDN_STEPS') or 999)
            _go = lambda n: n <= _lim
            for i in range(int(_os_env('GDN_NT') or NTILE)):
                tsl = slice(i * 128, (i + 1) * 128)
                qTt = [View(qkvh[h], qkv.t[:, h, tsl]) for h in range(4)]
                kTt = [View(qkvh[4 + h], qkv.t[:, 4 + h, tsl]) for h in range(4)]
                vTt = [View(qkvh[8 + h], qkv.t[:, 8 + h, tsl]) for h in range(4)]
                col = lambda tab, h: tab[:, i * 4 + h:i * 4 + h + 1]
                for h in (range(4) if _go(1) else ()):
                    d = H[h]
                    _b = int(_os_env('GDN_S1') or 255)
                    if _b & 1:
                        P.tr(d['pb'][0], kTt[h], identb[:, :])
                    if _b & 2:
                        P.tr(d['pb'][1], vTt[h], identb[:, :])
                    if _b & 4:
                        P.act(d['bgk'][:, :], d['pb'][0], AF.Copy, scale=col(bge, h))
                    if _b & 8:
                        P.ts('dve', d['kd0'][:, :], d['pb'][0], col(edl0, h), ALU.mult)
                    if _b & 16:
                        P.ts('dve', d['kd1'][:, :], d['pb'][0], col(edl1, h), ALU.mult)
                    if _b & 32:
                        P.act(d['bv'][:, :], d['pb'][1], AF.Copy, scale=col(betc, h))
                    if _b & 64:
                        P.ts('pool', d['GU'][:, :], U2, col(gcol, h), ALU.mult, 1.0, ALU.mult)
                    if _b & 128:
                        P.ts('pool', d['nG'][:, :], cm[:, C_ONES, :], col(ng, h), ALU.mult, 1.0, ALU.mult)
                for h in (range(4) if _go(2) else ()):
                    d = H[h]
                    P.mm(d['ps'][0], d['GU'][:, :], B2, start=True, stop=False)
                    P.mm(d['ps'][0], d['nG'][:, :], U2, start=False, stop=True)
                    P.mm(d['ps'][1], kTt[h], kTt[h])
                    P.mm(d['ps'][4], kTt[h], qTt[h])
                for h in (range(4) if _go(3) else ()):
                    d = H[h]
                    P.ts('dve', d['Dm'][:, :], d['ps'][0], 0.0, ALU.min)
                    P.ts('dve', d['Dn'][:, :], d['ps'][0], -1.0, ALU.mult, 0.0, ALU.min)
                    P.act(d['dec'][:, :], d['Dm'][:, :], AF.Exp)
                    P.act(d['decT'][:, :], d['Dn'][:, :], AF.Exp)
                    P.tt('pool', d['dec'][:, :], d['dec'][:, :], SL2, ALU.mult)
                    P.tt('pool', d['decT'][:, :], d['decT'][:, :], U2, ALU.mult)
                    P.stt(d['N'][:, :], d['ps'][1], col(nbeta, h), d['dec'][:, :], ALU.mult, ALU.mult)
                    P.tt('dve', d['qkm'][:, :], d['ps'][4], d['decT'][:, :], ALU.mult)
                for h in (range(4) if _go(4) else ()):
                    d = H[h]
                    P.tr(d['ps'][2], d['N'][:, :], IDf)
                for h in (range(4) if _go(5) else ()):
                    d = H[h]
                    P.cp('act', d['pTA'][:, :], d['ps'][2])
                    P.tt('dve', d['tT'][:, :], d['ps'][2], IDf, ALU.add)
                for h in (range(4) if _go(6) else ()):
                    H[h]['p'], H[h]['pT'], H[h]['pn'], H[h]['pTn'] = H[h]['N'], H[h]['pTA'], H[h]['pA'], H[h]['pTB']
                for it in range(5):
                    for h in (range(4) if _go(7) else ()):
                        d = H[h]
                        P.mm(d['ps'][2], d['pT'][:, :], d['p'][:, :])
                        if it < 4:
                            P.mm(d['ps'][3], d['p'][:, :], d['pT'][:, :])
                    for h in (range(4) if _go(8) else ()):
                        d = H[h]
                        P.cp('act', d['pn'][:, :], d['ps'][2])
                        if it < 4:
                            P.cp('dve', d['pTn'][:, :], d['ps'][3])
                    for h in (range(4) if _go(9) else ()):
                        d = H[h]
                        P.mm(d['ps'][4], d['pn'][:, :], d['tT'][:, :])
                    for h in (range(4) if _go(10) else ()):
                        d = H[h]
                        P.tt('dve', d['tT'][:, :], d['tT'][:, :], d['ps'][4], ALU.add)
                        oldp, oldpT = d['p'], d['pT']
                        d['p'], d['pT'] = d['pn'], d['pTn']
                        d['pn'] = d['pB'] if d['p'] is d['pA'] else d['pA']
                        d['pTn'] = d['pTA'] if d['pT'] is d['pTB'] else d['pTB']
                for h in (range(4) if _go(11) else ()):
                    d = H[h]
                    P.cp('act', d['TTb'][:, :], d['tT'][:, :])
                for h in (range(4) if _go(12) else ()):
                    d = H[h]
                    P.mm(d['ps'][0], d['TTb'][:, :], d['bv'][:, :])
                    P.mm(d['ps'][1], d['bgk'][:, :], d['TTb'][:, :])
                for h in (range(4) if _go(13) else ()):
                    d = H[h]
                    P.cp('act', d['usb'][:, :], d['ps'][0])
                    P.cp('dve', d['wT'][:, :], d['ps'][1])
                for c in range(2):
                    r = slice(c * 64, (c + 1) * 64)
                    for h in (range(4) if _go(14) else ()):
                        d = H[h]
                        P.mm(d['ps'][5], d['wT'][:, :], d['Sbf'][:, :])
                        P.mm(d['ps'][6], qTt[h], d['Sbf'][:, :])
                    for h in (range(4) if _go(15) else ()):
                        d = H[h]
                        P.tt('dve', d['vnew'][r, :], d['usb'][r, :], d['ps'][5][r, :], ALU.subtract)
                        P.act(d['osb'][r, :], d['ps'][6][r, :], AF.Copy, scale=View(egc, egc.t[r, i * 4 + h:i * 4 + h + 1]))
                    for h in (range(4) if _go(16) else ()):
                        d = H[h]
                        P.mm(d['ps'][0], d['qkm'][:, :], d['vnew'][:, :])
                        P.mm(d['ps'][1], d['kd%d' % c][:, :], d['vnew'][:, :])
                    for h in (range(4) if _go(17) else ()):
                        d = H[h]
                        P.tt('dve', d['osb'][r, :], d['osb'][r, :], d['ps'][0][r, :], ALU.add)
                        P.stt(d['S'][:, :], d['S'][:, :], EGL[:, c, i * 4 + h:i * 4 + h + 1], d['ps'][1], ALU.mult, ALU.add)
                        P.cp('act', d['Sbf'][:, :], d['S'][:, :])
                for h in (range(4) if _go(18) else ()):
                    d = H[h]
                    P.dma('sp', d['zt'][:, :], ptv[:, 12 + h, tsl])
                    P.act(d['osq'][:, :], d['osb'][:, :], AF.Square, accum=d['ss'][:, :])
                    P.act(d['rr'][:, :], d['ss'][:, :], AF.Sqrt, bias=1e-6, scale=1.0 / 128.0)
                    P.recip(d['rr'][:, :], d['rr'][:, :])
                    P.ts('dve', d['on'][:, :], d['osb'][:, :], d['rr'][:, 0:1], ALU.mult)
                    P.act(d['zs'][:, :], d['zt'][:, :], AF.Silu)
                for h in (range(4) if _go(19) else ()):
                    d = H[h]
                    P.tr(d['pb'][0], d['on'][:, :], identb[:, :])
                for h in (range(4) if _go(20) else ()):
                    d = H[h]
                    P.stt(d['og'][:, :], d['pb'][0], gnw[:, 0:1], d['zs'][:, :], ALU.mult, ALU.mult)
                    P.dma('sp', gdv[:, h, tsl], d['og'][:, :])

    def moba_stage(l, PT, MO):
        with P.stage():
            ptv = PT.re('(c p) t -> p c t', p=128)
            qb_ = P.sbuf([128, 2, L], BF16)
            kb_ = P.sbuf([128, 2, L], BF16)
            vb_ = P.sbuf([128, 2, L], BF16)
            P.dma('sp', qb_[:, :, :], ptv[:, 18:20, :])
            P.dma('sp', kb_[:, :, :], ptv[:, 20:22, :])
            P.dma('sp', vb_[:, :, :], ptv[:, 22:24, :])
            cmf = P.sbuf([128, 2, 256], F32)
            cmk = P.sbuf([128, 2, 256], BF16)
            P.dma('sp', cmf[:, :, :], cmaskD[:, :, :])
            P.cp('dve', cmk[:, :, :], cmf[:, :, :])
            stf = P.sbuf([128, 16, 128], F32)
            selT = P.sbuf([128, 16, 128], BF16)
            qz = P.sbuf([128, 4, L], BF16)
            for h in range(4):
                P.ts('pool' if h % 2 else 'dve', qz[:, h, :], qb_[:, h // 2, :], cv[:, (h % 2):(h % 2) + 1], ALU.mult)
            P.dma('sp', stf[:, :, :], selTD[:, :, :])
            P.cp('dve', selT[:, :, :], stf[:, :, :])
            psb = P.psum([128, 1024], BF16)
            psbs = [psb, psb]
            Vtm = P.sbuf([128, NTILE, 260], BF16)
            P.memset('pool', Vtm[:, :, :], 1.0)
            k = 0
            for i in range(NTILE):
                for c in range(2):
                    pb = psbs[k % 2]
                    po = (k % 2) * 128
                    k += 1
                    P.tr(View(pb, psb.t[:, po:po + 128]), vb_[:, c, i * 128:(i + 1) * 128], identb[:, :])
                    for hh in range(2):
                        h = 2 * c + hh
                        P.cp('act' if hh else 'dve', Vtm[:, i, h * 65:h * 65 + 64],
                             View(pb, psb.t[:, po + hh * 64:po + (hh + 1) * 64]))
            kmT = P.sbuf([128, 2, 16], F32)
            for c in range(2):
                P.op('dve', lambda e, c=c: e.tensor_reduce(out=kmT.t[:, c, :], in_=kb_.t[:, c, :].rearrange('p (n s) -> p n s', s=256),
                                                           axis=AX.X, op=ALU.add), reads=[kb_], writes=[kmT])
            P.ts('dve', kmT[:, :, :], kmT[:, :, :], 1.0 / 256.0, ALU.mult)
            psmisc = P.psum([128, 512])
            psg = psmisc
            pstf = psmisc
            psS = P.psum([128, 512])
            psST = [P.psum([128, 512]) for _ in range(2)]
            psO = [P.psum([128, 512]) for _ in range(2)]
            qf = P.sbuf([128, 4, 128], F32)
            gpad = P.sbuf([128, 4, 16], F32)
            top8 = P.sbuf([128, 4, 8], F32)
            selm = P.sbuf([128, 4, 16], F32)
            bias = P.sbuf([128, 4, 16], F32)
            mx = P.sbuf([128, 4, 8], F32)
            mrow = P.sbuf([128, 4], F32)
            biasT = [P.sbuf([128, 4, 256], BF16) for _ in range(2)]
            for b_ in biasT:
                P.memset('pool', b_[:, :, :], 0.0)
            PTs = [P.sbuf([128, 256], BF16) for _ in range(2)]
            rden = P.sbuf([128, 1], F32)
            Otm = P.sbuf([128, NTILE, 256], BF16)
            kk = 0
            for qbi in range(16):
                bT = biasT[qbi % 2]
                for half in range(2):
                    i = 2 * qbi + half
                    tsl = slice(i * 128, (i + 1) * 128)
                    P.cp('pool', qf[:, :, :], qz[:, :, tsl])
                    P.memset('pool', selm[:, :, :], 0.0)
                    if qbi > 0:
                        for h in range(4):
                            c, r0 = h // 2, (h % 2) * 64
                            P.mm(View(psg, psmisc.t[:, h * 16:(h + 1) * 16]), qf[:, h, :], kmT[:, c, :])
                        if qbi <= 3:
                            P.memset('pool', selm[:, :, 0:qbi], 1.0)
                        else:
                            P.memset('dve', gpad[:, :, :], -1e30)
                            P.cp('dve', gpad[:, :, 0:qbi],
                                 View(psg, psmisc.t[:, 0:64].rearrange('p (h n) -> p h n', n=16)[:, :, 0:qbi]))
                            for h in range(4):
                                P.op('dve', lambda e, h=h: e.max(out=top8.t[:, h, :], in_=gpad.t[:, h, :]),
                                     reads=[gpad], writes=[top8])
                                P.ts('dve', selm[:, h, 0:qbi], gpad[:, h, 0:qbi], top8[:, h, 2:3], ALU.is_ge)
                    P.memset('pool', selm[:, :, qbi:qbi + 1], 1.0)
                    nj = (qbi + 2) // 2
                    for h in range(4):
                        c, r0 = h // 2, (h % 2) * 64
                        for j in range(nj):
                            P.mm(psS[:, :], qz[:, h, tsl], kb_[:, c, j * 512:(j + 1) * 512])
                            P.op('dve', lambda e, h=h, j=j: e.reduce_max(out=mx.t[:, h, j:j + 1], in_=psS.t[:, :], axis=AX.X),
                                 reads=[psS], writes=[mx])
                        P.op('dve', lambda e, h=h, nj=nj: e.reduce_max(out=mrow.t[:, h:h + 1], in_=mx.t[:, h, 0:nj], axis=AX.X),
                             reads=[mx], writes=[mrow])
                    P.ts('dve', bias[:, :, :], selm[:, :, :], BIGRAW, ALU.mult, -BIGRAW, ALU.add)
                    P.tt('dve', bias[:, :, :], bias[:, :, :], mrow[:, :].re('p (h o) -> p h o', o=1).bc([128, 4, 16]), ALU.subtract)
                    for h in range(4):
                        P.tr(View(pstf, psmisc.t[0:16, 128:256]), bias[:, h, :], cm[:, C_ID, :])
                        P.cp('act', bT[0:16, h, half * 128:(half + 1) * 128], View(pstf, psmisc.t[0:16, 128:256]))
                qsl = slice(qbi * 256, (qbi + 1) * 256)
                for h in range(4):
                    c, r0 = h // 2, (h % 2) * 64
                    nst = 2 * (qbi + 1)
                    for st_ in range(nst):
                        kb, sc = st_ // 2, st_ % 2
                        ps = psST[kk % 2]
                        pt = PTs[kk % 2]
                        kk += 1
                        own = (kb == qbi)
                        P.mm(ps[:, 0:256], kb_[:, c, st_ * 128:(st_ + 1) * 128], qz[:, h, qsl],
                             start=True, stop=False)
                        P.mm(ps[:, 0:256], selT[:, kb, :], bT[:, h, :], start=False, stop=(not own))
                        if own:
                            P.mm(ps[:, 0:256], identb[:, :], cmk[:, sc, :], start=False, stop=True)
                        P.act(pt[:, :], ps[:, 0:256], AF.Exp, scale=0.125)
                        for th in range(2):
                            P.mm(psO[th][:, 0:65], pt[:, th * 128:(th + 1) * 128], Vtm[:, st_, h * 65:(h + 1) * 65],
                                 start=(st_ == 0), stop=(st_ == nst - 1))
                    for th in range(2):
                        P.recip(rden[:, :], psO[th][:, 64:65])
                        P.ts('dve', Otm[:, 2 * qbi + th, h * 64:(h + 1) * 64], psO[th][:, 0:64], rden[:, 0:1], ALU.mult)
            MOsb = P.sbuf([128, 2, L], BF16)
            k = 0
            for i in range(NTILE):
                for c in range(2):
                    pb = psbs[k % 2]
                    po = (k % 2) * 128
                    k += 1
                    P.tr(View(pb, psb.t[:, po:po + 128]), Otm[:, i, c * 128:(c + 1) * 128], identb[:, :])
                    P.cp('act' if c else 'dve', MOsb[:, c, i * 128:(i + 1) * 128], View(pb, psb.t[:, po:po + 128]))
            P.dma('sp', MO.re('(c p) t -> p c t', p=128), MOsb[:, :, :])

    def peer_cast_stage(l, UTb, Vb):
        w = W[l]
        with P.stage():
            stg = [P.sbuf([128, 8, 512], F32) for _ in range(2)]
            ob = [P.sbuf([128, 8, 512], BF16) for _ in range(2)]
            uv = w['uT'].re('(kc p) e -> p kc e', p=128)
            uo = UTb.re('(kc p) e -> p kc e', p=128)
            vv = w['vtab'].re('(ec p) d -> p ec d', p=128)
            vo = Vb.re('(ec p) d -> p ec d', p=128)
            k = 0
            engs = ['pool', 'act', 'dve']
            for pc in range(32):
                st, o_ = stg[k % 2], ob[k % 2]
                P.dma('sp', st[:, :, :], uv[:, :, pc * 512:(pc + 1) * 512])
                P.cp(engs[k % 3], o_[:, :, :], st[:, :, :])
                P.dma('sp', uo[:, :, pc * 512:(pc + 1) * 512], o_[:, :, :])
                k += 1
            for pc in range(32):
                st, o_ = stg[k % 2], ob[k % 2]
                sv = View(st, st.t[:, :, :].rearrange('p a (b c) -> p (a b) c', b=1)) if False else None
                stv = View(st, st.t[:, :, :].rearrange('p a b -> p (a b)').rearrange('p (a b) -> p a b', a=4))
                obv = View(o_, o_.t[:, :, :].rearrange('p a b -> p (a b)').rearrange('p (a b) -> p a b', a=4))
                P.dma('sp', stv, vv[:, pc * 4:(pc + 1) * 4, :])
                P.cp(engs[k % 3], o_[:, :, :], st[:, :, :])
                P.dma('sp', vo[:, pc * 4:(pc + 1) * 4, :], obv)
                k += 1

    def peer_prep_stage(l, Xin, H2, QS):
        w = W[l]
        with P.stage():
            hT = P.sbuf([128, 8, L], BF16)
            hblk = [hT.alias() for _ in range(8)]
            norm_mod(Xin, A2[l], mod[l][:, 24:32], hblk, hT)
            h2v = H2.re('(kc p) t -> p kc t', p=128)
            for tb in range(8):
                P.dma('sp', h2v[:, :, tb * 512:(tb + 1) * 512], View(hblk[tb], hT.t[:, :, tb * 512:(tb + 1) * 512]))
            wstg = [P.sbuf([128, 8, 512], F32) for _ in range(2)]
            wblk = [P.sbuf([128, 8, 512], BF16) for _ in range(2)]
            ots = [P.sbuf([128, 4, 512], F32) for _ in range(2)]
            pss = [P.psum([128, 512]) for _ in range(4)]
            wv = w['wq'].re('(kc p) j -> p kc j', p=128)
            qsv = QS.re('(c p) t -> p c t', p=128)
            k = 0
            for cb in range(4):
                wb = wblk[cb % 2]
                P.dma('sp', wstg[cb % 2][:, :, :], wv[:, :, cb * 512:(cb + 1) * 512])
                P.cp('pool', wb[:, :, :], wstg[cb % 2][:, :, :])
                for tb in range(8):
                    ot = ots[(cb * 8 + tb) % 2]
                    for j in range(4):
                        ps = pss[k % 4]
                        k += 1
                        for kc in range(8):
                            P.mm(ps[:, :], wb[:, kc, j * 128:(j + 1) * 128],
                                 View(hblk[tb], hT.t[:, kc, tb * 512:(tb + 1) * 512]), start=(kc == 0), stop=(kc == 7))
                        P.cp('act' if k % 2 else 'dve', ot[:, j, :], ps[:, :])
                    P.dma('sp', qsv[:, cb * 4:(cb + 1) * 4, tb * 512:(tb + 1) * 512], ot[:, :, :])

    def peer_stage(l, Xin, Xout, H2, QS, UTb, Vb, ntb=16):
        w = W[l]
        EBS = 1024
        NI1 = EBS // 128
        NEB = 16384 // EBS
        with P.stage():
            k1T = P.sbuf([128, 8, 128], F32)
            k2T = P.sbuf([128, 8, 128], F32)
            P.dma('sp', k1T[:, :, :], w['k1T'].re('h d n -> d h n'))
            P.dma('sp', k2T[:, :, :], w['k2T'].re('h d n -> d h n'))
            h2v = H2.re('(kc p) t -> p kc t', p=128)
            qsv = QS.re('(c p) t -> p c t', p=128)
            xv = Xin.re('(kc p) t -> p kc t', p=128)
            ov = Xout.re('(kc p) t -> p kc t', p=128)
            utv = UTb.re('(kc p) e -> p kc e', p=128)
            vbv = Vb.re('(ec p) d -> p ec d', p=128)
            hs = [P.sbuf([128, 8, 256], BF16) for _ in range(2)]
            qT = [P.sbuf([128, 16, 256], F32) for _ in range(2)]
            xts = [P.sbuf([128, 8, 256], F32) for _ in range(2)]
            ot = P.sbuf([128, 8, 256], F32)
            UT = [P.sbuf([128, 8, EBS], BF16) for _ in range(2)]
            VB = [P.sbuf([128, NI1, D], BF16) for _ in range(2)]
            E1 = P.sbuf([128, 2, 8, 128], F32)
            E2 = P.sbuf([128, 2, 8, 128], F32)
            TH = P.sbuf([128, 2, 8], F32)
            sc = [P.sbuf([128, 256], F32) for _ in range(2)]
            scr = P.sbuf([128, 256], F32)
            v12 = P.sbuf([128, 2, 16], F32)
            cand = P.sbuf([128, 16, 16], F32)
            cscr = P.sbuf([128, 256], F32)
            t16 = P.sbuf([128, 16], F32)
            junk = P.sbuf([128, 16], F32)
            e1t = P.sbuf([128, 128], F32)
            sm1 = lambda: P.sbuf([128, 1], F32)
            nm1, nm2, nm, Z, thr = sm1(), sm1(), sm1(), sm1(), sm1()
            Wsum = [P.sbuf([128, NI1, 128], BF16) for _ in range(2)]
            wtmp = [P.sbuf([128, NI1, 128], F32) for _ in range(2)]
            wtmp2 = P.sbuf([128, NI1, 128], BF16)
            gel = [P.sbuf([128, 512], BF16) for _ in range(2)]
            Wg = [P.sbuf([128, 512], BF16) for _ in range(2)]
            WgT = [P.sbuf([128, 4, 128], BF16) for _ in range(2)]
            Ysb = P.sbuf([128, D], F32)
            psY = [[P.psum([128, 512]) for _ in range(2)] for _ in range(2)]
            psA = [P.psum([128, 512]) for _ in range(2)]
            psT = P.psum([128, 1024], BF16)
            psM = P.psum([128, 512])
            flat = lambda b: View(b, b.t[:, :, :].rearrange('p a b -> p (a b)'))
            ka = 0
            for tb in range(ntb):
                tsl = slice(tb * 256, (tb + 1) * 256)
                h_, q_, xt = hs[tb % 2], qT[tb % 2], xts[tb % 2]
                P.dma('sp', h_[:, :, :], h2v[:, :, tsl])
                P.dma('sp', q_[:, :, :], qsv[:, :, tsl])
                P.dma('sp', xt[:, :, :], xv[:, :, tsl])
                for tile in range(2):
                    cs_ = slice(tile * 128, (tile + 1) * 128)
                    for h in range(8):
                        s_ = sc[h % 2]
                        P.mm(psM[:, 0:128], q_[:, 2 * h, cs_], k1T[:, h, :])
                        P.mm(psM[:, 128:256], q_[:, 2 * h + 1, cs_], k2T[:, h, :])
                        P.cp('act', s_[:, :], psM[:, 0:256])
                        for hf in range(2):
                            o = hf * 128
                            P.op('dve', lambda e, hf=hf, o=o, s_=s_: e.max(out=v12.t[:, hf, 0:8], in_=s_.t[:, o:o + 128]),
                                 reads=[s_], writes=[v12])
                            P.op('dve', lambda e, hf=hf, o=o, s_=s_: e.match_replace(
                                out=scr.t[:, o:o + 128], in_to_replace=v12.t[:, hf, 0:8], in_values=s_.t[:, o:o + 128],
                                imm_value=-1e30), reads=[s_, v12], writes=[scr])
                            P.op('dve', lambda e, hf=hf, o=o: e.max(out=v12.t[:, hf, 8:16], in_=scr.t[:, o:o + 128]),
                                 reads=[scr], writes=[v12])
                        P.tt('dve', cand[:, :, :], v12[:, 0, :].re('p (a o) -> p a o', o=1).bc([128, 16, 16]),
                             v12[:, 1, :].re('p (o b) -> p o b', o=1).bc([128, 16, 16]), ALU.add)
                        P.op('dve', lambda e: e.max(out=t16.t[:, 0:8], in_=cand.t[:, :, :].rearrange('p a b -> p (a b)')),
                             reads=[cand], writes=[t16])
                        P.op('dve', lambda e: e.match_replace(out=cscr.t[:, :], in_to_replace=t16.t[:, 0:8],
                                                              in_values=cand.t[:, :, :].rearrange('p a b -> p (a b)'),
                                                              imm_value=-1e30), reads=[cand, t16], writes=[cscr])
                        P.op('dve', lambda e: e.max(out=t16.t[:, 8:16], in_=cscr.t[:, :]), reads=[cscr], writes=[t16])
                        P.ts('dve', nm1[:, :], v12[:, 0, 0:1], -1.0, ALU.mult)
                        P.ts('dve', nm2[:, :], v12[:, 1, 0:1], -1.0, ALU.mult)
                        P.ts('dve', nm[:, :], t16[:, 0:1], -1.0, ALU.mult)
                        P.act(junk[:, :], t16[:, :], AF.Exp, bias=nm[:, 0:1], accum=Z[:, :])
                        P.recip(Z[:, :], Z[:, :])
                        P.act(E2[:, tile, h, :], s_[:, 128:256], AF.Exp, bias=nm2[:, 0:1])
                        P.act(e1t[:, :], s_[:, 0:128], AF.Exp, bias=nm1[:, 0:1])
                        P.ts('dve', E1[:, tile, h, :], e1t[:, :], Z[:, 0:1], ALU.mult)
                        P.ts('dve', nm[:, :], nm[:, :], -1e-3, ALU.add)
                        P.act(thr[:, :], t16[:, 15:16], AF.Exp, bias=nm[:, 0:1])
                        P.tt('dve', TH[:, tile, h:h + 1], thr[:, :], Z[:, :], ALU.mult)
                for eb in range(NEB):
                    ut, vb = UT[eb % 2], VB[eb % 2]
                    P.dma('sp', ut[:, :, :], utv[:, :, eb * EBS:(eb + 1) * EBS])
                    P.dma('sp', vb[:, :, :], vbv[:, eb * NI1:(eb + 1) * NI1, :])
                    for tile in range(2):
                        cs_ = slice(tile * 128, (tile + 1) * 128)
                        ws = Wsum[tile]
                        for h in range(8):
                            wt = wtmp[h % 2]
                            P.tt('pool', wt[:, :, :],
                                 E1[:, tile, h, eb * NI1:(eb + 1) * NI1].re('p (a o) -> p a o', o=1).bc([128, NI1, 128]),
                                 E2[:, tile, h, :].re('p (o b) -> p o b', o=1).bc([128, NI1, 128]), ALU.mult)
                            if h == 0:
                                P.stt(flat(ws), flat(wt), TH[:, tile, h:h + 1], flat(wt), ALU.is_ge, ALU.mult)
                            else:
                                P.stt(flat(wtmp2), flat(wt), TH[:, tile, h:h + 1], flat(wt), ALU.is_ge, ALU.mult)
                                P.tt('dve', flat(ws), flat(ws), flat(wtmp2), ALU.add)
                        for sub in range(EBS // 512):
                            pa = psA[ka % 2]
                            g_, wg_, wgt_ = gel[ka % 2], Wg[ka % 2], WgT[ka % 2]
                            ka += 1
                            for kc in range(8):
                                P.mm(pa[:, :], h_[:, kc, cs_], ut[:, kc, sub * 512:(sub + 1) * 512],
                                     start=(kc == 0), stop=(kc == 7))
                            P.act(g_[:, :], pa[:, :], AF.Gelu)
                            P.tt('dve', wg_[:, :], g_[:, :], flat(ws)[:, sub * 512:(sub + 1) * 512], ALU.mult)
                            for j in range(4):
                                P.tr(psT[:, j * 128:(j + 1) * 128], wg_[:, j * 128:(j + 1) * 128], identb[:, :])
                            P.cp('act', flat(wgt_), psT[:, 0:512])
                            for j in range(4):
                                ec = sub * 4 + j
                                first = (eb == 0 and sub == 0 and j == 0)
                                last = (eb == NEB - 1 and sub == EBS // 512 - 1 and j == 3)
                                for half in range(2):
                                    P.mm(psY[tile][half][:, :], wgt_[:, j, :], vb[:, ec, half * 512:(half + 1) * 512],
                                         start=first, stop=last)
                for tile in range(2):
                    cs_ = slice(tile * 128, (tile + 1) * 128)
                    P.cp('act', Ysb[:, 0:512], psY[tile][0][:, :])
                    P.cp('dve', Ysb[:, 512:1024], psY[tile][1][:, :])
                    for dc in range(8):
                        pa = psA[dc % 2]
                        P.tr(pa[:, 0:128], Ysb[:, dc * 128:(dc + 1) * 128], cm[:, C_ID, :])
                        P.stt(ot[:, dc, cs_], pa[:, 0:128], mod[l][:, 40 + dc:41 + dc], xt[:, dc, cs_], ALU.mult, ALU.add)
                P.dma('sp', ov[:, :, tsl], ot[:, :, :])

    def merge_stage(l, PT, GD, S5O, MO, Xin, Xout, branches):
        w = W[l]
        TBM = 256
        with P.stage():
            wstg = P.sbuf([128, 8, D], F32)

            def loadw(src, nk):
                f = P.sbuf([128, nk, D], BF16)
                P.dma('sp', wstg[:, 0:nk, :], src.re('(kc p) j -> p kc j', p=128))
                P.cp('pool', f[:, :, :], wstg[:, 0:nk, :])
                return f
            wa, wb_, wc, wo = loadw(w['wa'], 4), loadw(w['wb'], 2), loadw(w['wc'], 2), loadw(w['wout'], 8)
            ptv = PT.re('(c p) t -> p c t', p=128)
            xv = Xin.re('(kc p) t -> p kc t', p=128)
            ov = Xout.re('(kc p) t -> p kc t', p=128)
            srcs = [(GD, 4, wa), (S5O, 2, wb_), (MO, 2, wc)]
            bts = [[P.sbuf([128, nk, TBM], BF16) for _ in range(2)] for (_, nk, _) in srcs]
            gts = [P.sbuf([128, 24, TBM], BF16) for _ in range(2)]
            xts = [P.sbuf([128, 8, TBM], F32) for _ in range(2)]
            ots = [P.sbuf([128, 8, TBM], F32) for _ in range(2)]
            mg = P.sbuf([128, 8, TBM], BF16)
            acc = P.sbuf([128, TBM], F32)
            tmp = P.sbuf([128, TBM], F32)
            pss = [P.psum([128, 512]) for _ in range(6)]
            k = 0
            for tb in range(L // TBM):
                tsl = slice(tb * TBM, (tb + 1) * TBM)
                for bi, (src, nk, _) in enumerate(srcs):
                    if bi in branches:
                        P.dma('sp', bts[bi][tb % 2][:, :, :], src.re('(c p) t -> p c t', p=128)[:, :, tsl])
                gt = gts[tb % 2]
                P.dma('sp', gt[:, :, :], ptv[:, 24:48, tsl])
                xt = xts[tb % 2]
                P.dma('sp', xt[:, :, :], xv[:, :, tsl])
                for dc in range(8):
                    first = True
                    for bi, (src, nk, wt) in enumerate(srcs):
                        if bi not in branches:
                            continue
                        ps = pss[k % 6]
                        k += 1
                        for kc in range(nk):
                            P.mm(ps[:, 0:TBM], wt[:, kc, dc * 128:(dc + 1) * 128], bts[bi][tb % 2][:, kc, :],
                                 start=(kc == 0), stop=(kc == nk - 1))
                        if first:
                            P.tt('dve', acc[:, :], ps[:, 0:TBM], gt[:, bi * 8 + dc, :], ALU.mult)
                            first = False
                        else:
                            P.tt('dve', tmp[:, :], ps[:, 0:TBM], gt[:, bi * 8 + dc, :], ALU.mult)
                            P.tt('pool', acc[:, :], acc[:, :], tmp[:, :], ALU.add)
                    P.cp('act', mg[:, dc, :], acc[:, :])
                ot = ots[tb % 2]
                for oc in range(8):
                    ps = pss[k % 6]
                    k += 1
                    for kc in range(8):
                        P.mm(ps[:, 0:TBM], wo[:, kc, oc * 128:(oc + 1) * 128], mg[:, kc, :], start=(kc == 0), stop=(kc == 7))
                    P.stt(ot[:, oc, :], ps[:, 0:TBM], mod[l][:, 16 + oc:17 + oc], xt[:, oc, :], ALU.mult, ALU.add)
                P.dma('sp', ov[:, :, tsl], ot[:, :, :])

    if peer_only:
        Xc = ein('XMin', [D, L])
        UTb = P.dram('UTb0', [D, 16384], BF16)
        Vb = P.dram('Vb0', [16384, D], BF16)
        H2 = P.dram('H2_0', [D, L], BF16)
        QS = dbg_dram('QS0', [2048, L], F32)
        Xo = dbg_dram('XO0', [D, L], F32)
        peer_cast_stage(0, UTb, Vb)
        peer_prep_stage(0, Xc, H2, QS)
        peer_stage(0, Xc, Xo, H2, QS, UTb, Vb, ntb=peer_ntb)
        P.close()
        return nc, dbg_out
    Xcur = xT
    for l in range(2):
        if upto < 1:
            break
        PT = dbg_dram('PT%d' % l, [48 * 128, L], BF16)
        baTM = P.sbuf([128, NTILE, 8], F32, persist=True, name='baTM%d' % l)
        if fake_pt and l == 0:
            PT = ein('PTin', [48 * 128, L], BF16)
            baIn = ein('baTMin', [128, NTILE, 8])
            with P.stage():
                P.dma('sp', baTM[:, :, :], baIn[:, :, :])
        with (contextlib.nullcontext() if (fake_pt and l == 0) else P.stage()):
          if not (fake_pt and l == 0):
              hT = P.sbuf([128, 8, L], BF16)
              hblk = [hT.alias() for _ in range(8)]
              norm_mod(Xcur, A1[l], mod[l][:, 0:8], hblk, hT)
              if 'h1' in dbg and l == 0:
                  dh = dbg_dram('h1', [128, 8, L], BF16)
                  P.dma('sp', dh[:, :, :], View(hT, hT.t[:, :, :]))
              wblk = [P.sbuf([128, 8, 512], BF16) for _ in range(2)]
              wstg = [P.sbuf([128, 8, 512], F32) for _ in range(2)]
              ots = [P.sbuf([128, 4, 512], BF16) for _ in range(2)]
              pss = [P.psum([128, 512]) for _ in range(4)]
              wv = W[l]['w_main'].re('(kc p) j -> p kc j', p=128)
              ptv = PT.re('(c p) t -> p c t', p=128)
              k = 0
              for cb in range(12):
                  wb = wblk[cb % 2]
                  P.dma('sp', wstg[cb % 2][:, :, :], wv[:, :, cb * 512:(cb + 1) * 512])
                  P.cp('pool', wb[:, :, :], wstg[cb % 2][:, :, :])
                  for tb in range(8):
                      ot = ots[(cb * 8 + tb) % 2]
                      for j in range(4):
                          ps = pss[k % 4]
                          k += 1
                          for kc in range(8):
                              P.mm(ps[:, :], wb[:, kc, j * 128:(j + 1) * 128],
                                   View(hblk[tb], hT.t[:, kc, tb * 512:(tb + 1) * 512]),
                                   start=(kc == 0), stop=(kc == 7))
                          chunk = cb * 4 + j
                          if chunk >= 24:
                              P.act(ot[:, j, :], ps[:, :], AF.Sigmoid)
                          elif k % 2 == 0:
                              P.cp('act', ot[:, j, :], ps[:, :])
                          else:
                              P.cp('dve', ot[:, j, :], ps[:, :])
                      P.dma('sp', ptv[:, cb * 4:(cb + 1) * 4, tb * 512:(tb + 1) * 512], ot[:, :, :])
              wbaf = P.sbuf([128, 8, 8], F32)
              wba = P.sbuf([128, 8, 8], BF16)
              P.dma('sp', wbaf[:, :, :], W[l]['w_ba'].re('(kc p) j -> p kc j', p=128))
              P.cp('dve', wba[:, :, :], wbaf[:, :, :])
              for i in range(NTILE):
                  ps = pss[i % 4]
                  for kc in range(8):
                      P.mm(ps[:, 0:8], View(hblk[i // 4], hT.t[:, kc, i * 128:(i + 1) * 128]), wba[:, kc, :],
                           start=(kc == 0), stop=(kc == 7))
                  P.cp('dve', baTM[:, i, :], ps[:, 0:8])
              if 'ba' in dbg and l == 0:
                  db = dbg_dram('ba', [128, NTILE, 8], F32)
                  P.dma('sp', db[:, :, :], baTM[:, :, :])
        if upto < 2:
            break
        S5O = dbg_dram('S5O%d' % l, [256, L], BF16)
        GD = dbg_dram('GD%d' % l, [512, L], BF16)
        MO = dbg_dram('MO%d' % l, [256, L], BF16)
        if 0 in branches:
            GQ = dbg_dram('GQ%d' % l, [12 * 128, L], BF16)
            gdn_prep_stage(l, PT, GQ)
            if not _os_env('GDN_PREP_ONLY'):
                gdn_stage(l, PT, GQ, GD, baTM)
        if 1 in branches:
            s5_stage(l, PT, S5O)
        if 2 in branches:
            moba_stage(l, PT, MO)
        if upto < 3:
            break
        Xn = dbg_dram('XM%d' % l, [D, L], F32)
        merge_stage(l, PT, GD, S5O, MO, Xcur, Xn, branches)
        Xcur = Xn
        if upto < 4:
            break
        if 3 in branches:
            if fake_xm and l == 0:
                Xcur = ein('XMin', [D, L])
            UTb = P.dram('UTb%d' % l, [D, 16384], BF16)
            Vb = P.dram('Vb%d' % l, [16384, D], BF16)
            H2 = P.dram('H2_%d' % l, [D, L], BF16)
            QS = dbg_dram('QS%d' % l, [2048, L], F32)
            Xo = dbg_dram('XO%d' % l, [D, L], F32)
            peer_cast_stage(l, UTb, Vb)
            peer_prep_stage(l, Xcur, H2, QS)
            peer_stage(l, Xcur, Xo, H2, QS, UTb, Vb, ntb=peer_ntb)
            Xcur = Xo

    with P.stage():
        fw = P.sbuf([128, 8], F32)
        P.dma('sp', fw[:, :], fnwT[:, :])
        xv = Xcur.re('(kc p) t -> p kc t', p=128)
        ov = outT.re('(kc p) t -> p kc t', p=128)
        xts = [P.sbuf([128, 8, 512], F32) for _ in range(2)]
        ots = [P.sbuf([128, 8, 512], F32) for _ in range(2)]
        sq = P.sbuf([128, 8, 512], F32)
        rs = P.sbuf([128, 512], F32)
        ssp = P.psum([128, 512])
        for tb in range(8):
            xt = xts[tb % 2]
            ot = ots[tb % 2]
            P.dma('sp', xt[:, :, :], xv[:, :, tb * 512:(tb + 1) * 512])
            P.act(sq[:, :, :], xt[:, :, :], AF.Square)
            for kc in range(8):
                P.mm(ssp[:, :], cm[:, C_ONES, :], sq[:, kc, :], start=(kc == 0), stop=(kc == 7))
            P.act(rs[:, :], ssp[:, :], AF.Sqrt, bias=1e-6, scale=1.0 / D)
            P.recip(rs[:, :], rs[:, :])
            for kc in range(8):
                P.stt(ot[:, kc, :], xt[:, kc, :], fw[:, kc:kc + 1], rs[:, :], ALU.mult, ALU.mult)
            P.dma('sp', ov[:, :, tb * 512:(tb + 1) * 512], ot[:, :, :])
    P.close()
    return nc, dbg_out


def prep_inputs(inp, b):
    f = lambda a: np.ascontiguousarray(a, dtype=np.float32)
    m = {}
    m['xT'] = f(inp['x'][b].T)
    m['cT'] = f(inp['c'][b].reshape(8, 128).T)
    m['cmats'] = _const_mats()
    cv = np.zeros((128, 4), np.float32)
    cv[:64, 0] = 1.0
    cv[64:, 1] = 1.0
    m['cvec'] = cv
    m['fnwT'] = f(inp['final_norm_w'].reshape(8, 128).T)
    pp = np.arange(128)[:, None, None]
    scc = np.arange(2)[None, :, None]
    tl = np.arange(256)[None, None, :]
    m['cmaskD'] = np.where(tl >= scc * 128 + pp, 0.0, -BIGRAW).astype(np.float32)
    st = np.zeros((128, 16, 128), np.float32)
    for n in range(16):
        st[n, n, :] = 1.0
    m['selTD'] = st
    for l in range(2):
        s = '_%d' % l
        m['ada_w' + s] = f(inp['ada_w'][l])
        m['ada_bT' + s] = f(inp['ada_b'][l].reshape(48, 128).T)
        m['n1T' + s] = f(inp['norm1_w'][l].reshape(8, 128).T)
        m['n2T' + s] = f(inp['norm2_w'][l].reshape(8, 128).T)
        wi = inp['w_in'][l]
        m['w_main' + s] = f(np.concatenate([wi[:, 0:2048], wi[:, 2056:6152]], axis=1))
        m['w_ba' + s] = f(wi[:, 2048:2056])
        q = np.arange(128)
        gi = 2 * np.arange(8)[None, :] + (q // 64)[:, None]
        pi = np.broadcast_to((q % 64)[:, None], (128, 8))
        m['s5_are' + s] = f(inp['s5_a_re'][l][gi, pi])
        m['s5_aim' + s] = f(inp['s5_a_im'][l][gi, pi])
        m['s5_ldt' + s] = f(inp['s5_log_dt'][l][gi])
        m['s5_bre' + s] = f(inp['s5_b_re'][l][gi, pi])
        m['s5_bim' + s] = f(inp['s5_b_im'][l][gi, pi])
        m['s5_ctre' + s] = f(inp['s5_c_re'][l].transpose(0, 2, 1))
        m['s5_ctim' + s] = f(inp['s5_c_im'][l].transpose(0, 2, 1))
        m['s5_dT' + s] = f(inp['s5_d'][l].reshape(2, 128).T)
        m['s5_gluw' + s] = f(inp['s5_glu_w'][l])
        m['s5_glubT' + s] = f(inp['s5_glu_b'][l].reshape(4, 128).T)
        m['wq' + s] = f(inp['peer_wq'][l])
        m['k1T' + s] = f(inp['peer_k1'][l].transpose(0, 2, 1))
        m['k2T' + s] = f(inp['peer_k2'][l].transpose(0, 2, 1))
        m['uT' + s] = f(inp['peer_u'][l].T)
        m['vtab' + s] = f(inp['peer_v'][l])
        m['convT' + s] = f(inp['gdn_conv_w'][l].reshape(4, 12, 128).transpose(2, 1, 0))
        m['gdn_ab' + s] = f(np.broadcast_to(np.concatenate([inp['gdn_a_log'][l], inp['gdn_dt_bias'][l]])[None, :], (128, 8)))
        m['gnw' + s] = f(inp['gdn_norm_w'][l].reshape(128, 1))
        m['wa' + s] = f(inp['w_branch_a'][l])
        m['wb' + s] = f(inp['w_branch_b'][l])
        m['wc' + s] = f(inp['w_branch_c'][l])
        m['wout' + s] = f(inp['w_out'][l])
    return m


def kernel(**inputs):
    nc, _ = build()
    in_maps = [prep_inputs(inputs, b) for b in range(8)]
    res = run_bass_kernel_spmd(nc, in_maps, core_ids=list(range(8)))
    out = np.stack([res.results[b]['outT'].T for b in range(8)], axis=0)
    return np.ascontiguousarray(out.astype(np.float32))
```
